# Optimizing a Trainium2 kernel written in Bass

```python
import math
import jax, jax.numpy as jnp
from jax import lax
import numpy as np

D_MODEL = 1024
BATCH = 32
SEQ = 2048
DEPTH = 1

POOL_WINDOWS = (2, 4, 8, 16)
N_POOL_GROUPS = len(POOL_WINDOWS)
POOL_WIDTH = D_MODEL // 2
POOL_GROUP = POOL_WIDTH // N_POOL_GROUPS
N_HEADS = 4
HEAD_DIM = D_MODEL // 16
V_HEAD_DIM = 2 * HEAD_DIM
QK_WIDTH = N_HEADS * 2 * HEAD_DIM
ATTN_WIDTH = N_HEADS * V_HEAD_DIM
Q_BLOCK = 128
OFF_POOL = 0
OFF_Q = OFF_POOL + POOL_WIDTH
OFF_K = OFF_Q + QK_WIDTH
OFF_V = OFF_K + QK_WIDTH
OFF_GP = OFF_V + ATTN_WIDTH
OFF_GA = OFF_GP + D_MODEL
IN_WIDTH = OFF_GA + D_MODEL
N_GROUPS = 4
EXPERTS_PER_GROUP = 8
N_EXPERTS = N_GROUPS * EXPERTS_PER_GROUP
TOP_K = 2
D_EXPERT = D_MODEL // 2
ROW_BLOCK = 512
EPS = 1e-6

kernel_name = "hybrid_pool_diffattn_hmoe_block"


def rmsnorm(x, g):
    xf = x.astype(jnp.float32)
    y = xf * lax.rsqrt(jnp.mean(xf * xf, axis=-1, keepdims=True) + EPS)
    return (y * g.astype(jnp.float32)).astype(x.dtype)


def lambda_init(layer):
    return 0.8 - 0.6 * math.exp(-0.3 * layer)


def alibi_slopes():
    return jnp.asarray([2.0 ** (-8.0 * (h + 1) / N_HEADS) for h in range(N_HEADS)], jnp.float32)


def pool_mixer(u, w_grp, scale):
    B, S, C = u.shape
    uf = u.astype(jnp.float32)
    c0 = jnp.concatenate([jnp.zeros((B, 1, C), jnp.float32), jnp.cumsum(uf, axis=1)], axis=1)
    t = jnp.arange(S)
    outs = []
    for g, w in enumerate(POOL_WINDOWS):
        sl = slice(g * POOL_GROUP, (g + 1) * POOL_GROUP)
        cg = c0[:, :, sl]
        upper = cg[:, 1:]
        lower = jnp.pad(cg[:, :S + 1 - w], ((0, 0), (w - 1, 0), (0, 0)))
        cnt = jnp.minimum(t + 1, w).astype(jnp.float32)[None, :, None]
        outs.append((upper - lower) / cnt - uf[:, :, sl])
    d = jnp.stack(outs, axis=2).astype(u.dtype)
    y = jnp.einsum('bsgc,gcd->bsgd', d, w_grp).reshape(B, S, POOL_WIDTH)
    return y * scale


def diff_attention(q, k, v, q_g, k_g, lam, subln_g, lam_init):
    B, S = q.shape[0], q.shape[1]
    qf = rmsnorm(q.astype(jnp.float32), q_g) * (HEAD_DIM ** -0.5)
    kf = rmsnorm(k.astype(jnp.float32), k_g)
    slopes = alibi_slopes()
    nb = S // Q_BLOCK
    kpos = jnp.arange(S)
    qb = qf.reshape(B, nb, Q_BLOCK, N_HEADS, 2, HEAD_DIM).transpose(1, 0, 2, 3, 4, 5)

    def block(args):
        qblk, i = args
        qpos = i * Q_BLOCK + jnp.arange(Q_BLOCK)
        dist = (qpos[:, None] - kpos[None, :]).astype(jnp.float32)
        bias = jnp.where(dist[None] >= 0, -slopes[:, None, None] * dist[None], -jnp.inf)
        s = jnp.einsum('bqhmd,bkhmd->bhmqk', qblk, kf) + bias[None, :, None]
        p = jax.nn.softmax(s, axis=-1)
        a = p[:, :, 0] - lam * p[:, :, 1]
        return jnp.einsum('bhqk,bkhe->bqhe', a.astype(v.dtype), v)

    o = lax.map(block, (qb, jnp.arange(nb)))
    o = o.transpose(1, 0, 2, 3, 4).reshape(B, S, N_HEADS, V_HEAD_DIM)
    o = rmsnorm(o, subln_g) * (1.0 - lam_init)
    return o.reshape(B, S, ATTN_WIDTH)


def hier_moe(hn, w_rg, b_rg, w_re, b_re, w_gate, w_up, w_down):
    N, D = hn.shape
    pg = jax.nn.softmax((hn @ w_rg).astype(jnp.float32) + b_rg.astype(jnp.float32), axis=-1)
    p_top, g_idx = lax.top_k(pg, 1)
    le = ((hn @ w_re).astype(jnp.float32) + b_re.astype(jnp.float32)).reshape(N, N_GROUPS, EXPERTS_PER_GROUP)
    le_sel = jnp.take_along_axis(le, g_idx[:, :, None], axis=1)[:, 0]
    pe = jax.nn.softmax(le_sel, axis=-1)
    pv, e_idx = lax.top_k(pe, TOP_K)
    wts = p_top * pv / jnp.sum(pv, axis=-1, keepdims=True)
    eid = (g_idx * EXPERTS_PER_GROUP + e_idx).reshape(-1)
    tok = jnp.repeat(jnp.arange(N, dtype=jnp.int32), TOP_K)
    wflat = wts.reshape(-1)
    M = N * TOP_K
    order = jnp.argsort(eid)
    s_eid, s_tok, s_w = eid[order], tok[order], wflat[order]
    counts = jnp.bincount(eid, length=N_EXPERTS)
    padded = (counts + ROW_BLOCK - 1) // ROW_BLOCK * ROW_BLOCK
    pend = jnp.cumsum(padded)
    pstart = pend - padded
    start = jnp.cumsum(counts) - counts
    dest = pstart[s_eid] + jnp.arange(M) - start[s_eid]
    n_blocks = -(-M // ROW_BLOCK) + N_EXPERTS
    P = n_blocks * ROW_BLOCK
    slot_tok = jnp.zeros((P,), jnp.int32).at[dest].set(s_tok)
    slot_w = jnp.zeros((P,), jnp.float32).at[dest].set(s_w)
    block_eid = jnp.minimum(jnp.searchsorted(pend, jnp.arange(n_blocks) * ROW_BLOCK, side='right'), N_EXPERTS - 1)

    def expert_block(args):
        toks, wb, e = args
        xb = hn[toks]
        hdn = jax.nn.silu(xb @ w_gate[e]) * (xb @ w_up[e])
        return (hdn @ w_down[e]) * wb[:, None].astype(hn.dtype)

    yb = lax.map(expert_block, (slot_tok.reshape(n_blocks, ROW_BLOCK), slot_w.reshape(n_blocks, ROW_BLOCK), block_eid))
    return jnp.zeros((N, D), hn.dtype).at[slot_tok].add(yb.reshape(P, D))


def setup_inputs(seed: int = 0) -> dict:
    key = jax.random.key(seed)
    ks = jax.random.split(key, 24)
    f32 = jnp.float32
    nrm = lambda k, shape, fan: jax.random.normal(k, shape, f32) * (fan ** -0.5)
    gain = lambda k, shape: 1.0 + 0.02 * jax.random.normal(k, shape, f32)
    L = DEPTH
    return {
        "x": jax.random.normal(ks[0], (BATCH, SEQ, D_MODEL), f32),
        "norm1_g": gain(ks[1], (L, D_MODEL)),
        "w_in": nrm(ks[2], (L, D_MODEL, IN_WIDTH), D_MODEL),
        "pool_w": nrm(ks[3], (L, N_POOL_GROUPS, POOL_GROUP, POOL_GROUP), POOL_GROUP),
        "pool_scale": gain(ks[4], (L, POOL_WIDTH)),
        "w_pool_up": nrm(ks[5], (L, POOL_WIDTH, D_MODEL), POOL_WIDTH),
        "q_norm_g": gain(ks[6], (L, HEAD_DIM)),
        "k_norm_g": gain(ks[7], (L, HEAD_DIM)),
        "lambda_q1": 0.1 * jax.random.normal(ks[8], (L, HEAD_DIM), f32),
        "lambda_k1": 0.1 * jax.random.normal(ks[9], (L, HEAD_DIM), f32),
        "lambda_q2": 0.1 * jax.random.normal(ks[10], (L, HEAD_DIM), f32),
        "lambda_k2": 0.1 * jax.random.normal(ks[11], (L, HEAD_DIM), f32),
        "subln_g": gain(ks[12], (L, V_HEAD_DIM)),
        "w_attn_up": nrm(ks[13], (L, ATTN_WIDTH, D_MODEL), ATTN_WIDTH),
        "w_out": nrm(ks[14], (L, D_MODEL, D_MODEL), D_MODEL),
        "norm2_g": gain(ks[15], (L, D_MODEL)),
        "w_router_group": nrm(ks[16], (L, D_MODEL, N_GROUPS), D_MODEL),
        "b_router_group": 0.01 * jax.random.normal(ks[17], (L, N_GROUPS), f32),
        "w_router_expert": nrm(ks[18], (L, D_MODEL, N_EXPERTS), D_MODEL),
        "b_router_expert": 0.01 * jax.random.normal(ks[19], (L, N_EXPERTS), f32),
        "w_expert_gate": nrm(ks[20], (L, N_EXPERTS, D_MODEL, D_EXPERT), D_MODEL),
        "w_expert_up": nrm(ks[21], (L, N_EXPERTS, D_MODEL, D_EXPERT), D_MODEL),
        "w_expert_down": nrm(ks[22], (L, N_EXPERTS, D_EXPERT, D_MODEL), D_EXPERT),
    }


def reference(x, norm1_g, w_in, pool_w, pool_scale, w_pool_up, q_norm_g, k_norm_g, lambda_q1, lambda_k1,
              lambda_q2, lambda_k2, subln_g, w_attn_up, w_out, norm2_g, w_router_group, b_router_group,
              w_router_expert, b_router_expert, w_expert_gate, w_expert_up, w_expert_down):
    B, S, D = x.shape
    for l in range(DEPTH):
        lam_init = lambda_init(l)
        h = rmsnorm(x, norm1_g[l])
        proj = h @ w_in[l]
        u = proj[..., OFF_POOL:OFF_Q]
        q = proj[..., OFF_Q:OFF_K].reshape(B, S, N_HEADS, 2, HEAD_DIM)
        k = proj[..., OFF_K:OFF_V].reshape(B, S, N_HEADS, 2, HEAD_DIM)
        v = proj[..., OFF_V:OFF_GP].reshape(B, S, N_HEADS, V_HEAD_DIM)
        gate_p = jax.nn.sigmoid(proj[..., OFF_GP:OFF_GA])
        gate_a = jax.nn.sigmoid(proj[..., OFF_GA:IN_WIDTH])
        pool_out = pool_mixer(u, pool_w[l], pool_scale[l]) @ w_pool_up[l]
        lam = (jnp.exp(jnp.sum(lambda_q1[l].astype(jnp.float32) * lambda_k1[l].astype(jnp.float32)))
               - jnp.exp(jnp.sum(lambda_q2[l].astype(jnp.float32) * lambda_k2[l].astype(jnp.float32))) + lam_init)
        attn_out = diff_attention(q, k, v, q_norm_g[l], k_norm_g[l], lam, subln_g[l], lam_init) @ w_attn_up[l]
        x = x + (gate_p * pool_out + gate_a * attn_out) @ w_out[l]
        hn = rmsnorm(x, norm2_g[l]).reshape(B * S, D)
        x = x + hier_moe(hn, w_router_group[l], b_router_group[l], w_router_expert[l], b_router_expert[l],
                         w_expert_gate[l], w_expert_up[l], w_expert_down[l]).reshape(B, S, D)
    return x
```

```python
import math
from contextlib import ExitStack
import numpy as np
import ml_dtypes
import concourse.bass as bass
import concourse.mybir as mybir
from concourse.bass_utils import run_bass_kernel_spmd

F32 = mybir.dt.float32
BF16 = mybir.dt.bfloat16
I32 = mybir.dt.int32
ALU = mybir.AluOpType
AF = mybir.ActivationFunctionType
AX = mybir.AxisListType

N_CORES = 8
D = 1024
SEQ = 2048
SEQ_PER_CORE = 4
TPS = SEQ // 128
NT = SEQ_PER_CORE * TPS
NTOK = NT * 128
IN_W = 4096
NE = 32
DE = 512
CAP = 640
CT = CAP // 128
NSLOT = NE * CAP
EPS = 1e-6
LAM_INIT = 0.8 - 0.6 * math.exp(0.0)
BIG = 1.0e4
LOOKAHEAD = 3


class Buf:
    __slots__ = ("name", "w", "r")

    def __init__(self, name):
        self.name = name
        self.w = None
        self.r = {}


class K:
    def __init__(self, nc):
        self.nc = nc
        self.eng = {"pe": nc.tensor, "act": nc.scalar, "dve": nc.vector, "pool": nc.gpsimd, "sp": nc.sync}
        self.sems = {}
        self.cnt = {}
        self.waited = {}
        self._stack = []
        self.pool_fifo = []
        for e in ("pe", "act", "dve", "pool"):
            self.newsem(e)

    def newsem(self, key):
        cm = self.nc.semaphore("s_" + key)
        h = cm.__enter__()
        self._stack.append(cm)
        self.sems[key] = h
        self.cnt[key] = 0
        return key

    def close(self):
        for cm in reversed(self._stack):
            cm.__exit__(None, None, None)

    def _wait(self, e, key, val):
        if self.waited.get((e, key), 0) >= val:
            return
        self.waited[(e, key)] = val
        self.eng[e].wait_ge(self.sems[key], val)

    def _deps(self, e, reads, writes):
        deps = {}

        def add(tok, skip_same):
            if tok is None:
                return
            kk, v = tok
            if kk == e and skip_same:
                return
            if deps.get(kk, 0) < v:
                deps[kk] = v
        for b in reads:
            add(b.w, e == "pe")
        for b in writes:
            add(b.w, e == "pe")
            for kk, v in b.r.items():
                add((kk, v), e == "pe")
        for kk, v in deps.items():
            self._wait(e, kk, v)

    def _record(self, tok, reads, writes):
        for b in reads:
            if b.r.get(tok[0], 0) < tok[1]:
                b.r[tok[0]] = tok[1]
        for b in writes:
            b.w = tok
            b.r = {}

    def op(self, e, ins_fn, reads=(), writes=(), inc=True):
        self._deps(e, reads, writes)
        ins = ins_fn()
        if inc:
            self.cnt[e] += 1
            ins.then_inc(self.sems[e], 1)
            tok = (e, self.cnt[e])
        else:
            tok = (e, self.cnt[e] + 1)
        self._record(tok, reads, writes)
        return ins

    def dma(self, q, semkey, ins_fn, reads=(), writes=()):
        self._deps(q, reads, writes)
        ins = ins_fn()
        self.cnt[semkey] += 16
        ins.then_inc(self.sems[semkey], 16)
        self._record((semkey, self.cnt[semkey]), reads, writes)
        return ins

    def barrier(self, engines=("pe", "act", "dve", "pool", "sp")):
        for e in engines:
            for kk, v in self.cnt.items():
                if v > 0 and kk != e:
                    self._wait(e, kk, v)


class _StopTile(Exception):
    pass


def build_program(n_seq=SEQ_PER_CORE, phases="ABC", nt_lim=None, stop_at=None):
    nc = bass.Bass("TRN2", target_bir_lowering=False)
    NT = n_seq * TPS
    NTOK = NT * 128
    NT_A = NT if nt_lim is None else nt_lim

    def din(name, shape, dt=F32):
        return nc.dram_tensor(name, list(shape), dt, kind="ExternalInput").ap()

    x = din("x", [NTOK, D])
    norm1_g = din("norm1_g", [1, D])
    w_in = din("w_in", [D, IN_W])
    pool_w = din("pool_w", [4, 128, 128])
    pool_scale = din("pool_scale", [1, 512])
    w_pool_up = din("w_pool_up", [512, D])
    q_norm_g = din("q_norm_g", [1, 64])
    k_norm_g = din("k_norm_g", [1, 64])
    lq1 = din("lambda_q1", [1, 64])
    lk1 = din("lambda_k1", [1, 64])
    lq2 = din("lambda_q2", [1, 64])
    lk2 = din("lambda_k2", [1, 64])
    subln_g = din("subln_g", [1, 128])
    w_attn_up = din("w_attn_up", [512, D])
    w_out = din("w_out", [D, D])
    norm2_g = din("norm2_g", [1, D])
    w_rg = din("w_router_group", [D, 4])
    b_rg = din("b_router_group", [1, 4])
    w_re = din("w_router_expert", [D, NE])
    b_re = din("b_router_expert", [1, NE])
    NE_decl = NE if "B" in phases else 1
    w_eg = din("w_expert_gate", [NE_decl, D, DE])
    w_eu = din("w_expert_up", [NE_decl, D, DE])
    w_ed = din("w_expert_down", [NE_decl, DE, D])
    c_poolA = din("c_poolA", [12, 128, 128])
    c_alibi = din("c_alibi", [128, 64])
    c_cmask = din("c_cmask", [128, 128])
    c_ustrict = din("c_ustrict", [128, 128])
    c_eoff = din("c_eoff", [128, NE])
    c_ident = din("c_ident", [128, 128])

    out = nc.dram_tensor("out", [NTOK, D], F32, kind="ExternalOutput").ap()
    xslots = nc.dram_tensor("xslots", [NSLOT, D], BF16, kind="Internal").ap()
    ybuf = nc.dram_tensor("ybuf", [NSLOT, D], F32, kind="Internal").ap()

    k = K(nc)
    V, S, P, G, T = nc.vector, nc.scalar, nc.tensor, nc.gpsimd, nc.sync
    bc_reg = G.to_reg(NSLOT - 1)

    with ExitStack() as top:
        def sbt(es, name, shape, dt):
            return es.enter_context(nc.sbuf_tensor(name, list(shape), dt))

        def pst(es, name, shape, dt):
            return es.enter_context(nc.psum_tensor(name, list(shape), dt))

        slot_all = sbt(top, "slot_all", [128, NT * 2], I32)
        w_all = sbt(top, "w_all", [128, NT * 2], F32)
        B_slot = [Buf(f"slot{i}") for i in range(NT)]
        B_wall = [Buf(f"wall{i}") for i in range(NT)]

        with ExitStack() as es:
            zt = sbt(es, "zt", [128, 8 * D], BF16)
            Bz = Buf("zt")
            k.newsem("zf")
            k.op("dve", lambda: V.memset(zt[:], 0.0), writes=[Bz])
            for r0 in range(0, NSLOT, 1024):
                k.dma("sp", "zf", lambda r0=r0: T.dma_start(out=xslots[r0:r0 + 1024, :].rearrange("(p a) n -> p (a n)", p=128), in_=zt[:]), reads=[Bz])
            k.barrier()

        with ExitStack() as es:
            w_in_bf = sbt(es, "w_in_bf", [128, 8, IN_W], BF16)
            w_out_bf = sbt(es, "w_out_bf", [128, 8, D], BF16)
            wpu_bf = sbt(es, "wpu_bf", [128, 4, D], BF16)
            wau_bf = sbt(es, "wau_bf", [128, 4, D], BF16)
            poolw_bf = sbt(es, "poolw_bf", [128, 4, 128], BF16)
            wr_f = sbt(es, "wr_f", [128, 8, 36], F32)
            g1_bc = sbt(es, "g1_bc", [128, D], F32)
            g2_bc = sbt(es, "g2_bc", [128, D], F32)
            gsub_bc = sbt(es, "gsub_bc", [128, 128], F32)
            gq_bc = sbt(es, "gq_bc", [128, 64], F32)
            gk_bc = sbt(es, "gk_bc", [128, 64], F32)
            lam_in = sbt(es, "lam_in", [128, 4, 64], F32)
            lam_w = sbt(es, "lam_w", [128, 8], F32)
            g2col = sbt(es, "g2col", [128, 8], F32)
            g2rows = sbt(es, "g2rows", [8, 128], F32)
            psrows = sbt(es, "psrows", [4, 128], F32)
            pscale = sbt(es, "pscale", [128, 4], F32)
            rbias = sbt(es, "rbias", [128, 36], F32)
            poolA_bf = sbt(es, "poolA_bf", [128, 12, 128], BF16)
            alibi = sbt(es, "alibi", [128, 64], F32)
            cmask_bf = sbt(es, "cmask_bf", [128, 128], BF16)
            ustrict_bf = sbt(es, "ustrict_bf", [128, 128], BF16)
            ones_bf = sbt(es, "ones_bf", [128, 128], BF16)
            eoff = sbt(es, "eoff", [128, NE], F32)
            ident_f = sbt(es, "ident_f", [128, 128], F32)
            ident_bf = sbt(es, "ident_bf", [128, 128], BF16)
            kT = sbt(es, "kT", [128, 4, SEQ], BF16)
            v_aug = sbt(es, "v_aug", [128, TPS, 4, 130], BF16)
            cum_bf = sbt(es, "cum_bf", [128, NE], BF16)
            xt = [sbt(es, f"xt{i}", [128, D], F32) for i in range(2)]
            eps_col = sbt(es, "eps_col", [128, 1], F32)
            st1 = sbt(es, "st1", [128, 8], F32)
            h_bf = sbt(es, "h_bf", [128, D], BF16)
            hT = sbt(es, "hT", [128, 8, 128], BF16)
            u_bf = [sbt(es, f"u_bf{i}", [128, 512], BF16) for i in range(2)]
            qkst = sbt(es, "qkst", [128, 16], F32)
            qn_bf = sbt(es, "qn_bf", [128, 512], BF16)
            kn_bf = sbt(es, "kn_bf", [128, 512], BF16)
            qT = sbt(es, "qT", [128, 4, 128], BF16)
            gp_sb = sbt(es, "gp_sb", [128, D], F32)
            ga_sb = sbt(es, "ga_sb", [128, D], F32)
            dT_bf = sbt(es, "dT_bf", [128, 512], BF16)
            yT_bf = sbt(es, "yT_bf", [128, 512], BF16)
            NPT = 8
            PT = [sbt(es, f"PT{i}", [128, 128], BF16) for i in range(NPT)]
            o_all = sbt(es, "o_all", [128, 4, 128], F32)
            ost = sbt(es, "ost", [128, 16], F32)
            otmp = sbt(es, "otmp", [128, 128], F32)
            o_n = sbt(es, "o_n", [128, 512], BF16)
            o_nT = sbt(es, "o_nT", [128, 4, 128], BF16)
            t1 = sbt(es, "t1", [128, D], F32)
            t2 = sbt(es, "t2", [128, D], F32)
            mix_bf = sbt(es, "mix_bf", [128, D], BF16)
            junk = mix_bf
            sq = t1[:, 0:512]
            qtmp = t2[:, 0:512]
            mixT = sbt(es, "mixT", [128, 8, 128], BF16)
            x1 = [sbt(es, "x1_0", [128, D], F32)] * 2
            hn_bf = [sbt(es, f"hn_bf{i}", [128, D], BF16) for i in range(2)]
            x1T = sbt(es, "x1T", [128, 4, 128], F32)
            L = sbt(es, "L", [128, 36], F32)
            rt = sbt(es, "rt", [128, 16], F32)
            gone = sbt(es, "gone", [128, 4], F32)
            gexp = sbt(es, "gexp", [128, 4], F32)
            Lm = sbt(es, "Lm", [128, NE], F32)
            Lm2 = sbt(es, "Lm2", [128, NE], F32)
            one1 = sbt(es, "one1", [128, NE], F32)
            one2 = sbt(es, "one2", [128, NE], F32)
            oh_bf = sbt(es, "oh_bf", [128, NE], BF16)
            rk = sbt(es, "rk", [128, NE], F32)
            tmp32 = sbt(es, "tmp32", [128, NE], F32)
            slotf = sbt(es, "slotf", [128, 2], F32)

            ps_tp = pst(es, "ps_tp", [128, D], BF16)
            ps_mm = [pst(es, f"ps_mm{i}", [128, 512], F32) for i in range(2)]
            ps_sc = [pst(es, f"ps_sc{i}", [128, 512], F32) for i in range(2)]
            ps_o = [pst(es, f"ps_o{i}", [128, 512], F32) for i in range(2)]
            ps_misc = pst(es, "ps_misc", [128, 512], F32)
            ps_tpf = ps_misc

            names = ["otmp", "wts", "wts2", "junk", "st1", "h", "pstp", "hT", "sq", "qtmp", "qkst", "qn", "kn", "qT", "kT", "vaug",
                     "gp", "ga", "dT", "yT", "pstpf", "pssc", "psmisc", "oall", "ost", "on", "onT", "t1", "t2", "mix",
                     "mixT", "x1T", "L", "rt", "gone", "gexp", "Lm", "Lm2", "one1", "one2", "oh", "rk", "tmp32",
                     "slotf", "cum", "pslg", "psrk", "consts"]
            B = {n: Buf(n) for n in names}
            B_xt = [Buf("xt0"), Buf("xt1")]
            B_u = [Buf("u0"), Buf("u1")]
            B_x1 = [Buf("x1_0")] * 2
            B_hn = [Buf("hn0"), Buf("hn1")]
            B_mm = [Buf("mm0"), Buf("mm1")]
            B_PT = [Buf(f"PT{i}") for i in range(NPT)]
            B_sc = [Buf(f"sc{i}") for i in range(2)]
            B_pso = [Buf("pso0"), Buf("pso1")]
            for s_ in ("wlp", "wls", "xl0", "xl1", "xs0", "xs1", "sc0", "sc1"):
                k.newsem(s_)

            W = ()
            Cst = [B["consts"]]
            for q4 in range(4):
                k.dma("pool", "wlp", lambda q4=q4: G.dma_start(out=w_in_bf[:, :, q4 * 1024:(q4 + 1) * 1024], in_=w_in[:, q4 * 1024:(q4 + 1) * 1024].rearrange("(kc p) n -> p kc n", p=128)), writes=W)
            k.dma("pool", "wlp", lambda: G.dma_start(out=w_out_bf[:], in_=w_out.rearrange("(kc p) n -> p kc n", p=128)), writes=W)
            k.dma("pool", "wlp", lambda: G.dma_start(out=wpu_bf[:], in_=w_pool_up.rearrange("(kc p) n -> p kc n", p=128)), writes=W)
            k.dma("pool", "wlp", lambda: G.dma_start(out=wau_bf[:], in_=w_attn_up.rearrange("(kc p) n -> p kc n", p=128)), writes=W)
            k.dma("pool", "wlp", lambda: G.dma_start(out=poolw_bf[:], in_=pool_w.rearrange("g c d -> c g d")), writes=W)
            k.dma("pool", "wlp", lambda: G.dma_start(out=poolA_bf[:], in_=c_poolA.rearrange("a j t -> j a t")), writes=W)
            k.dma("pool", "wlp", lambda: G.dma_start(out=cmask_bf[:], in_=c_cmask), writes=W)
            k.dma("pool", "wlp", lambda: G.dma_start(out=ustrict_bf[:], in_=c_ustrict), writes=W)
            k.dma("pool", "wlp", lambda: G.dma_start(out=ident_bf[:], in_=c_ident), writes=W)
            k.dma("sp", "wls", lambda: T.dma_start(out=ident_f[:], in_=c_ident), writes=W)
            k.dma("sp", "wls", lambda: T.dma_start(out=alibi[:], in_=c_alibi), writes=W)
            k.dma("sp", "wls", lambda: T.dma_start(out=eoff[:], in_=c_eoff), writes=W)
            k.dma("sp", "wls", lambda: T.dma_start(out=wr_f[:, :, 0:4], in_=w_rg.rearrange("(kc p) n -> p kc n", p=128)), writes=W)
            k.dma("sp", "wls", lambda: T.dma_start(out=wr_f[:, :, 4:36], in_=w_re.rearrange("(kc p) n -> p kc n", p=128)), writes=W)
            k.dma("sp", "wls", lambda: T.dma_start(out=g1_bc[:], in_=norm1_g[0:1, :].to_broadcast([128, D])), writes=W)
            k.dma("sp", "wls", lambda: T.dma_start(out=g2_bc[:], in_=norm2_g[0:1, :].to_broadcast([128, D])), writes=W)
            k.dma("sp", "wls", lambda: T.dma_start(out=gsub_bc[:], in_=subln_g[0:1, :].to_broadcast([128, 128])), writes=W)
            k.dma("sp", "wls", lambda: T.dma_start(out=gq_bc[:], in_=q_norm_g[0:1, :].to_broadcast([128, 64])), writes=W)
            k.dma("sp", "wls", lambda: T.dma_start(out=gk_bc[:], in_=k_norm_g[0:1, :].to_broadcast([128, 64])), writes=W)
            for i, lv in enumerate((lq1, lk1, lq2, lk2)):
                k.dma("sp", "wls", lambda i=i, lv=lv: T.dma_start(out=lam_in[:, i, :], in_=lv[0:1, :].to_broadcast([128, 64])), writes=W)
            k.dma("sp", "wls", lambda: T.dma_start(out=g2rows[:], in_=norm2_g.rearrange("o (kc p) -> (o kc) p", p=128)), writes=W)
            k.dma("sp", "wls", lambda: T.dma_start(out=psrows[:], in_=pool_scale.rearrange("o (g p) -> (o g) p", p=128)), writes=W)
            k.dma("sp", "wls", lambda: T.dma_start(out=rbias[:, 0:4], in_=b_rg[0:1, :].to_broadcast([128, 4])), writes=W)
            k.dma("sp", "wls", lambda: T.dma_start(out=rbias[:, 4:36], in_=b_re[0:1, :].to_broadcast([128, NE])), writes=W)
            B["wts"].w = ("wlp", k.cnt["wlp"])
            B["wts2"].w = ("wls", k.cnt["wls"])
            W = [B["wts"], B["wts2"]]

            k.op("dve", lambda: V.memset(ones_bf[:], 1.0), writes=Cst)
            k.op("dve", lambda: V.memset(eps_col[:], EPS), writes=Cst)
            k.op("dve", lambda: V.memset(cum_bf[:], 0.0), writes=[B["cum"]])
            k.op("pool", lambda: G.memset(v_aug[:], 1.0), writes=[B["vaug"]])
            k.op("pool", lambda: G.memset(hn_bf[0][:], 0.0), writes=[B_hn[0]])
            k.op("pool", lambda: G.memset(hn_bf[1][:], 0.0), writes=[B_hn[1]])
            k.op("pe", lambda: P.transpose(ps_misc[:, 0:8], g2rows[:], ident_f[0:8, 0:8]), reads=W, writes=[B["psmisc"]])
            k.op("dve", lambda: V.tensor_copy(out=g2col[:], in_=ps_misc[:, 0:8]), reads=[B["psmisc"]], writes=Cst)
            k.op("pe", lambda: P.transpose(ps_misc[:, 0:4], psrows[:], ident_f[0:4, 0:4]), reads=W, writes=[B["psmisc"]])
            k.op("dve", lambda: V.tensor_copy(out=pscale[:], in_=ps_misc[:, 0:4]), reads=[B["psmisc"]], writes=Cst)
            for kc in range(8):
                k.op("dve", lambda kc=kc: V.tensor_scalar(out=wr_f[:, kc, :], in0=wr_f[:, kc, :], scalar1=g2col[:, kc:kc + 1],
                                                           scalar2=None, op0=ALU.mult), reads=W + Cst, writes=Cst)
            k.op("dve", lambda: V.tensor_tensor(out=gq_bc[:], in0=gq_bc[:], in1=gk_bc[:], op=ALU.mult), reads=W, writes=Cst)
            k.op("dve", lambda: V.tensor_scalar(out=gq_bc[:], in0=gq_bc[:], scalar1=0.125, scalar2=None, op0=ALU.mult), reads=Cst, writes=Cst)
            k.op("dve", lambda: V.tensor_scalar(out=gsub_bc[:], in0=gsub_bc[:], scalar1=1.0 - LAM_INIT, scalar2=None, op0=ALU.mult), reads=W, writes=Cst)
            k.op("dve", lambda: V.tensor_tensor(out=lam_in[:, 0, :], in0=lam_in[:, 0, :], in1=lam_in[:, 1, :], op=ALU.mult), reads=W, writes=Cst)
            k.op("dve", lambda: V.tensor_tensor(out=lam_in[:, 2, :], in0=lam_in[:, 2, :], in1=lam_in[:, 3, :], op=ALU.mult), reads=Cst, writes=Cst)
            k.op("dve", lambda: V.tensor_reduce(out=lam_w[:, 0:1], in_=lam_in[:, 0, :], axis=AX.X, op=ALU.add), reads=Cst, writes=Cst)
            k.op("dve", lambda: V.tensor_reduce(out=lam_w[:, 1:2], in_=lam_in[:, 2, :], axis=AX.X, op=ALU.add), reads=Cst, writes=Cst)
            k.op("act", lambda: S.activation(out=lam_w[:, 2:4], in_=lam_w[:, 0:2], func=AF.Exp), reads=Cst, writes=Cst)
            k.op("dve", lambda: V.tensor_tensor(out=lam_w[:, 4:5], in0=lam_w[:, 2:3], in1=lam_w[:, 3:4], op=ALU.subtract), reads=Cst, writes=Cst)
            k.op("dve", lambda: V.tensor_scalar(out=lam_w[:, 5:6], in0=lam_w[:, 4:5], scalar1=LAM_INIT, scalar2=-1.0, op0=ALU.add, op1=ALU.mult), reads=Cst, writes=Cst)
            nlam = lam_w[:, 5:6]
            CW = [B["wts"], B["wts2"], B["consts"]]

            def load_x(Tg):
                s_ = Tg % 2
                k.dma("sp", f"xl{s_}", lambda: T.dma_start(out=xt[s_][:], in_=x[Tg * 128:(Tg + 1) * 128, :]), writes=[B_xt[s_]])

            try:
                if stop_at == 0:
                    raise _StopTile
                load_x(0)
                def stage12(Tg):
                    t = Tg % TPS
                    s_ = Tg % 2
                    xs, Bx = xt[s_], B_xt[s_]
                    x1s, Bx1 = x1[s_], B_x1[s_]
                    if Tg + 1 < NT_A:
                        load_x(Tg + 1)
                    k.op("act", lambda: S.activation(out=junk[:], in_=xs[:], func=AF.Square, accum_out=st1[:, 0:1]), reads=[Bx], writes=[B["st1"], B["mix"]])
                    k.op("act", lambda: S.activation(out=st1[:, 1:2], in_=st1[:, 0:1], func=AF.Ln, scale=1.0 / D, bias=eps_col[:, 0:1]), reads=[B["st1"]], writes=[B["st1"]])
                    k.op("act", lambda: S.activation(out=st1[:, 1:2], in_=st1[:, 1:2], func=AF.Exp, scale=-0.5), reads=[B["st1"]], writes=[B["st1"]])
                    k.op("act", lambda: S.activation(out=t2[:], in_=xs[:], func=AF.Copy, scale=st1[:, 1:2]), reads=[Bx, B["st1"]], writes=[B["t2"]])
                    k.op("dve", lambda: V.tensor_tensor(out=h_bf[:], in0=t2[:], in1=g1_bc[:], op=ALU.mult), reads=[B["t2"]] + CW, writes=[B["h"]])
                    for kc in range(8):
                        k.op("pe", lambda kc=kc: P.transpose(ps_tp[:, kc * 128:(kc + 1) * 128], h_bf[:, kc * 128:(kc + 1) * 128], ident_bf[:]),
                             reads=[B["h"]] + CW, writes=[B["pstp"]], inc=(kc == 7))
                    k.op("act", lambda: S.copy(out=hT[:].rearrange("p a b -> p (a b)"), in_=ps_tp[:]), reads=[B["pstp"]], writes=[B["hT"]])
                    for cb in range(8):
                        mm, Bm = ps_mm[cb % 2], B_mm[cb % 2]
                        for kc in range(8):
                            k.op("pe", lambda kc=kc, cb=cb, mm=mm: P.matmul(mm[:], hT[:, kc, :], w_in_bf[:, kc, cb * 512:(cb + 1) * 512], start=(kc == 0), stop=(kc == 7)),
                                 reads=[B["hT"]] + CW, writes=[Bm], inc=(kc == 7))
                        if cb == 0:
                            k.op("dve", lambda mm=mm: V.tensor_copy(out=u_bf[s_][:], in_=mm[:]), reads=[Bm], writes=[B_u[s_]])
                        elif cb in (1, 2):
                            o8 = 0 if cb == 1 else 8
                            k.op("act", lambda mm=mm: S.activation(out=sq, in_=mm[:], func=AF.Square), reads=[Bm], writes=[B["t1"]])
                            k.op("dve", lambda o8=o8: V.tensor_reduce(out=qkst[:, o8:o8 + 8], in_=sq.rearrange("p (a b) -> p a b", b=64), axis=AX.X, op=ALU.add),
                                 reads=[B["t1"]], writes=[B["qkst"]])
                            k.op("act", lambda o8=o8: S.activation(out=qkst[:, o8:o8 + 8], in_=qkst[:, o8:o8 + 8], func=AF.Ln, scale=1.0 / 64, bias=eps_col[:, 0:1]), reads=[B["qkst"]], writes=[B["qkst"]])
                            k.op("act", lambda o8=o8: S.activation(out=qkst[:, o8:o8 + 8], in_=qkst[:, o8:o8 + 8], func=AF.Exp, scale=-0.5), reads=[B["qkst"]], writes=[B["qkst"]])
                            rb = qkst[:, o8:o8 + 8].unsqueeze(2).to_broadcast([128, 8, 64])
                            if cb == 1:
                                k.op("dve", lambda mm=mm, rb=rb: V.tensor_tensor(out=qtmp.rearrange("p (a b) -> p a b", b=64), in0=mm[:].rearrange("p (a b) -> p a b", b=64), in1=rb, op=ALU.mult),
                                     reads=[Bm, B["qkst"]], writes=[B["t2"]])
                                k.op("dve", lambda: V.tensor_tensor(out=qn_bf[:].rearrange("p (a b) -> p a b", b=64), in0=qtmp.rearrange("p (a b) -> p a b", b=64),
                                                                    in1=gq_bc[:].unsqueeze(1).to_broadcast([128, 8, 64]), op=ALU.mult),
                                     reads=[B["t2"]] + CW, writes=[B["qn"]])
                            else:
                                k.op("dve", lambda mm=mm, rb=rb: V.tensor_tensor(out=kn_bf[:].rearrange("p (a b) -> p a b", b=64), in0=mm[:].rearrange("p (a b) -> p a b", b=64), in1=rb, op=ALU.mult),
                                     reads=[Bm, B["qkst"]], writes=[B["kn"]])
                        elif cb == 3:
                            k.op("act", lambda mm=mm: S.copy(out=v_aug[:, t, :, 0:128], in_=mm[:].rearrange("p (a b) -> p a b", b=128)), reads=[Bm], writes=[B["vaug"]])
                        elif cb in (4, 5):
                            k.op("act", lambda mm=mm, cb=cb: S.activation(out=gp_sb[:, (cb - 4) * 512:(cb - 3) * 512], in_=mm[:], func=AF.Sigmoid), reads=[Bm], writes=[B["gp"]])
                        else:
                            k.op("act", lambda mm=mm, cb=cb: S.activation(out=ga_sb[:, (cb - 6) * 512:(cb - 5) * 512], in_=mm[:], func=AF.Sigmoid), reads=[Bm], writes=[B["ga"]])

                def mid(Tg):
                    t = Tg % TPS
                    s_ = Tg % 2
                    xs, Bx = xt[s_], B_xt[s_]
                    x1s, Bx1 = x1[s_], B_x1[s_]
                    for hh in range(4):
                        k.op("pe", lambda hh=hh: P.transpose(ps_tp[:, hh * 128:(hh + 1) * 128], qn_bf[:, hh * 128:(hh + 1) * 128], ident_bf[:]),
                             reads=[B["qn"]] + CW, writes=[B["pstp"]], inc=False)
                    for hh in range(4):
                        k.op("pe", lambda hh=hh: P.transpose(ps_tp[:, 512 + hh * 128:512 + (hh + 1) * 128], kn_bf[:, hh * 128:(hh + 1) * 128], ident_bf[:]),
                             reads=[B["kn"]] + CW, writes=[B["pstp"]], inc=(hh == 3))
                    k.op("dve", lambda: V.tensor_copy(out=qT[:].rearrange("p a b -> p (a b)"), in_=ps_tp[:, 0:512]), reads=[B["pstp"]], writes=[B["qT"]])
                    for hh in range(4):
                        k.op("dve", lambda hh=hh: V.tensor_copy(out=kT[:, hh, t * 128:(t + 1) * 128], in_=ps_tp[:, 512 + hh * 128:512 + (hh + 1) * 128]), reads=[B["pstp"]], writes=[B["kT"]])
                    def pool_step1():
                        for g in range(4):
                            aidx = g if t == 0 else 4 + g
                            k.op("pe", lambda g=g, aidx=aidx: P.matmul(ps_misc[:, g * 128:(g + 1) * 128], u_bf[s_][:, g * 128:(g + 1) * 128], poolA_bf[:, aidx, :], start=True, stop=(t == 0)),
                                 reads=[B_u[s_]] + CW, writes=[B["psmisc"]], inc=(t == 0 and g == 3))
                            if t > 0:
                                k.op("pe", lambda g=g: P.matmul(ps_misc[:, g * 128:(g + 1) * 128], u_bf[1 - s_][:, g * 128:(g + 1) * 128], poolA_bf[:, 8 + g, :], start=False, stop=True),
                                     reads=[B_u[1 - s_]] + CW, writes=[B["psmisc"]], inc=(g == 3))
                        k.op("dve", lambda: V.tensor_copy(out=dT_bf[:], in_=ps_misc[:]), reads=[B["psmisc"]], writes=[B["dT"]])
                    def pool_step2():
                        for g in range(4):
                            k.op("pe", lambda g=g: P.matmul(ps_misc[:, g * 128:(g + 1) * 128], poolw_bf[:, g, :], dT_bf[:, g * 128:(g + 1) * 128], start=True, stop=True),
                                 reads=[B["dT"]] + CW, writes=[B["psmisc"]], inc=(g == 3))
                        k.op("dve", lambda: V.tensor_tensor(out=yT_bf[:].rearrange("p (a b) -> p a b", b=128), in0=ps_misc[:].rearrange("p (a b) -> p a b", b=128),
                                                            in1=pscale[:].unsqueeze(2).to_broadcast([128, 4, 128]), op=ALU.mult),
                             reads=[B["psmisc"]] + CW, writes=[B["yT"]])
                    def pool_step3():
                        for nh in range(2):
                            for g in range(4):
                                k.op("pe", lambda g=g, nh=nh: P.matmul(ps_mm[nh][:], yT_bf[:, g * 128:(g + 1) * 128], wpu_bf[:, g, nh * 512:(nh + 1) * 512], start=(g == 0), stop=(g == 3)),
                                     reads=[B["yT"]] + CW, writes=[B_mm[nh]], inc=(g == 3))
                            k.op("dve", lambda nh=nh: V.tensor_tensor(out=t1[:, nh * 512:(nh + 1) * 512], in0=ps_mm[nh][:], in1=gp_sb[:, nh * 512:(nh + 1) * 512], op=ALU.mult),
                                 reads=[B["gp"], B_mm[nh]], writes=[B["t1"]])
                    pool_steps = [pool_step1, pool_step2, pool_step3]
                    chunks = []
                    for hh in range(4):
                        for m in range(2):
                            for c0 in range(0, t + 1, 4):
                                chunks.append((hh, m, list(range(c0, min(c0 + 4, t + 1)))))

                    def emit_scores(ci):
                        hh, m, jl = chunks[ci]
                        bk = ci % 2
                        for q_, j in enumerate(jl):
                            k.op("pe", lambda: P.matmul(ps_sc[bk][:, q_ * 128:(q_ + 1) * 128], kT[64 * m:64 * m + 64, hh, j * 128:(j + 1) * 128], qT[64 * m:64 * m + 64, hh, :], start=True, stop=True),
                                 reads=[B["kT"], B["qT"]], writes=[B_sc[bk]], inc=(q_ == len(jl) - 1))

                    emit_scores(0)
                    ptc = 0
                    for ci, (hh, m, jl) in enumerate(chunks):
                        if ci + 1 < len(chunks):
                            emit_scores(ci + 1)
                        bk = ci % 2
                        po, Bpo = ps_o[hh % 2], B_pso[hh % 2]
                        for q_, j in enumerate(jl):
                            pr = ptc % NPT
                            ptc += 1
                            dlt = t - j
                            k.op("act", lambda: S.activation(out=PT[pr][:], in_=ps_sc[bk][:, q_ * 128:(q_ + 1) * 128], func=AF.Exp, bias=alibi[:, hh * 16 + dlt:hh * 16 + dlt + 1], scale=1.0),
                                 reads=[B_sc[bk]] + CW, writes=[B_PT[pr]])
                            if j == t:
                                k.op("pool", lambda: G.tensor_tensor(out=PT[pr][:], in0=PT[pr][:], in1=cmask_bf[:], op=ALU.mult), reads=[B_PT[pr]] + CW, writes=[B_PT[pr]])
                            k.op("pe", lambda: P.matmul(po[:, m * 129:(m + 1) * 129], PT[pr][:], v_aug[:, j, hh, 0:129], start=(j == 0), stop=(j == t)),
                                 reads=[B_PT[pr], B["vaug"]], writes=[Bpo], inc=(j == t))
                        if ci < len(pool_steps):
                            pool_steps[ci]()
                        if m == 1 and jl[-1] == t:
                            pov = po[:, 0:258].rearrange("p (a b) -> p a b", b=129)
                            k.op("dve", lambda: V.reciprocal(out=ost[:, 0:2].unsqueeze(2), in_=pov[:, :, 128:129]), reads=[Bpo], writes=[B["ost"]])
                            k.op("dve", lambda: V.tensor_tensor(out=ost[:, 2:3], in0=ost[:, 1:2], in1=nlam, op=ALU.mult), reads=[B["ost"]] + CW, writes=[B["ost"]])
                            k.op("dve", lambda: V.tensor_scalar(out=o_all[:, hh, :], in0=po[:, 0:128], scalar1=ost[:, 0:1], scalar2=None, op0=ALU.mult),
                                 reads=[Bpo, B["ost"]], writes=[B["oall"]])
                            k.op("dve", lambda: V.tensor_scalar(out=otmp[:], in0=po[:, 129:257], scalar1=ost[:, 2:3], scalar2=None, op0=ALU.mult), reads=[Bpo, B["ost"]], writes=[B["otmp"]])
                            k.op("dve", lambda: V.tensor_tensor(out=o_all[:, hh, :], in0=o_all[:, hh, :], in1=otmp[:], op=ALU.add), reads=[B["otmp"], B["oall"]], writes=[B["oall"]])
                    for hh in range(4):
                        k.op("act", lambda hh=hh: S.activation(out=junk[:, 0:128], in_=o_all[:, hh, :], func=AF.Square, accum_out=ost[:, 4 + hh:5 + hh]),
                             reads=[B["oall"]], writes=[B["ost"], B["mix"]])
                    k.op("act", lambda: S.activation(out=ost[:, 8:12], in_=ost[:, 4:8], func=AF.Ln, scale=1.0 / 128, bias=eps_col[:, 0:1]), reads=[B["ost"]], writes=[B["ost"]])
                    k.op("act", lambda: S.activation(out=ost[:, 8:12], in_=ost[:, 8:12], func=AF.Exp, scale=-0.5), reads=[B["ost"]], writes=[B["ost"]])
                    for hh in range(4):
                        k.op("dve", lambda hh=hh: V.tensor_scalar(out=otmp[:], in0=o_all[:, hh, :], scalar1=ost[:, 8 + hh:9 + hh], scalar2=None, op0=ALU.mult), reads=[B["oall"], B["ost"]], writes=[B["otmp"]])
                        k.op("dve", lambda hh=hh: V.tensor_tensor(out=o_n[:, hh * 128:(hh + 1) * 128], in0=otmp[:], in1=gsub_bc[:], op=ALU.mult), reads=[B["otmp"]] + CW, writes=[B["on"]])
                    for hh in range(4):
                        k.op("pe", lambda hh=hh: P.transpose(ps_tp[:, hh * 128:(hh + 1) * 128], o_n[:, hh * 128:(hh + 1) * 128], ident_bf[:]),
                             reads=[B["on"]] + CW, writes=[B["pstp"]], inc=(hh == 3))
                    k.op("dve", lambda: V.tensor_copy(out=o_nT[:].rearrange("p a b -> p (a b)"), in_=ps_tp[:, 0:512]), reads=[B["pstp"]], writes=[B["onT"]])
                    for nh in range(2):
                        for hh in range(4):
                            k.op("pe", lambda hh=hh, nh=nh: P.matmul(ps_mm[nh][:], o_nT[:, hh, :], wau_bf[:, hh, nh * 512:(nh + 1) * 512], start=(hh == 0), stop=(hh == 3)),
                                 reads=[B["onT"]] + CW, writes=[B_mm[nh]], inc=(hh == 3))
                        k.op("dve", lambda nh=nh: V.tensor_tensor(out=t2[:, nh * 512:(nh + 1) * 512], in0=ps_mm[nh][:], in1=ga_sb[:, nh * 512:(nh + 1) * 512], op=ALU.mult),
                             reads=[B["ga"], B_mm[nh]], writes=[B["t2"]])
                    k.op("pool", lambda: G.tensor_tensor(out=mix_bf[:], in0=t1[:], in1=t2[:], op=ALU.add), reads=[B["t1"], B["t2"]], writes=[B["mix"]])
                    for kc in range(8):
                        k.op("pe", lambda kc=kc: P.transpose(ps_tp[:, kc * 128:(kc + 1) * 128], mix_bf[:, kc * 128:(kc + 1) * 128], ident_bf[:]),
                             reads=[B["mix"]] + CW, writes=[B["pstp"]], inc=(kc == 7))
                    k.op("act", lambda: S.copy(out=mixT[:].rearrange("p a b -> p (a b)"), in_=ps_tp[:]), reads=[B["pstp"]], writes=[B["mixT"]])
                    for nh in range(2):
                        for kc in range(8):
                            k.op("pe", lambda kc=kc, nh=nh: P.matmul(ps_mm[nh][:], mixT[:, kc, :], w_out_bf[:, kc, nh * 512:(nh + 1) * 512], start=(kc == 0), stop=(kc == 7)),
                                 reads=[B["mixT"]] + CW, writes=[B_mm[nh]], inc=(kc == 7))
                        k.op("dve", lambda nh=nh: V.tensor_tensor(out=x1s[:, nh * 512:(nh + 1) * 512], in0=ps_mm[nh][:], in1=xs[:, nh * 512:(nh + 1) * 512], op=ALU.add),
                             reads=[Bx, B_mm[nh]], writes=[Bx1])
                    k.dma("sp", f"xs{s_}", lambda: T.dma_start(out=out[Tg * 128:(Tg + 1) * 128, :], in_=x1s[:]), reads=[Bx1])
                    k.op("act", lambda: S.activation(out=junk[:], in_=x1s[:], func=AF.Square, accum_out=st1[:, 2:3]), reads=[Bx1], writes=[B["st1"], B["mix"]])
                    k.op("act", lambda: S.activation(out=st1[:, 3:4], in_=st1[:, 2:3], func=AF.Ln, scale=1.0 / D, bias=eps_col[:, 0:1]), reads=[B["st1"]], writes=[B["st1"]])
                    k.op("act", lambda: S.activation(out=st1[:, 3:4], in_=st1[:, 3:4], func=AF.Exp, scale=-0.5), reads=[B["st1"]], writes=[B["st1"]])
                    k.op("act", lambda: S.activation(out=t2[:], in_=x1s[:], func=AF.Copy, scale=st1[:, 3:4]), reads=[Bx1, B["st1"]], writes=[B["t2"]])
                    k.op("dve", lambda: V.tensor_tensor(out=hn_bf[s_][:], in0=t2[:], in1=g2_bc[:], op=ALU.mult), reads=[B["t2"]] + CW, writes=[B_hn[s_]])

                def routing(Tg):
                    t = Tg % TPS
                    s_ = Tg % 2
                    xs, Bx = xt[s_], B_xt[s_]
                    x1s, Bx1 = x1[s_], B_x1[s_]
                    ps_lg = ps_o[0][:, 300:336]
                    ps_rk = ps_o[1][:, 300:332]
                    for half in range(2):
                        for c4 in range(4):
                            kc = half * 4 + c4
                            k.op("pe", lambda kc=kc, c4=c4: P.transpose(ps_tpf[:, c4 * 128:(c4 + 1) * 128], x1s[:, kc * 128:(kc + 1) * 128], ident_f[:]),
                                 reads=[Bx1] + CW, writes=[B["psmisc"]], inc=(c4 == 3))
                        k.op("act", lambda: S.copy(out=x1T[:].rearrange("p a b -> p (a b)"), in_=ps_tpf[:]), reads=[B["psmisc"]], writes=[B["x1T"]])
                        for c4 in range(4):
                            kc = half * 4 + c4
                            k.op("pe", lambda kc=kc, c4=c4: P.matmul(ps_lg, x1T[:, c4, :], wr_f[:, kc, :], start=(kc == 0), stop=(kc == 7)),
                                 reads=[B["x1T"]] + CW, writes=[B["pslg"]], inc=(c4 == 3))
                    k.op("dve", lambda: V.tensor_scalar(out=L[:], in0=ps_lg, scalar1=st1[:, 3:4], scalar2=None, op0=ALU.mult), reads=[B["pslg"], B["st1"]], writes=[B["L"]])
                    k.op("dve", lambda: V.tensor_tensor(out=L[:], in0=L[:], in1=rbias[:], op=ALU.add), reads=[B["L"]] + CW, writes=[B["L"]])
                    R = [B["rt"]]
                    k.op("dve", lambda: V.tensor_reduce(out=rt[:, 0:1], in_=L[:, 0:4], axis=AX.X, op=ALU.max), reads=[B["L"]], writes=R)
                    k.op("dve", lambda: V.tensor_scalar(out=rt[:, 1:2], in0=rt[:, 0:1], scalar1=-1.0, scalar2=None, op0=ALU.mult), reads=R, writes=R)
                    k.op("act", lambda: S.activation(out=gexp[:], in_=L[:, 0:4], func=AF.Exp, bias=rt[:, 1:2], scale=1.0, accum_out=rt[:, 2:3]), reads=[B["L"]] + R, writes=[B["gexp"]] + R)
                    k.op("dve", lambda: V.reciprocal(out=rt[:, 3:4], in_=rt[:, 2:3]), reads=R, writes=R)
                    k.op("dve", lambda: V.tensor_scalar(out=gone[:], in0=L[:, 0:4], scalar1=rt[:, 0:1], scalar2=None, op0=ALU.is_equal), reads=[B["L"]] + R, writes=[B["gone"]])
                    k.op("dve", lambda: V.tensor_scalar(out=gone[:], in0=gone[:], scalar1=BIG, scalar2=-BIG, op0=ALU.mult, op1=ALU.add), reads=[B["gone"]], writes=[B["gone"]])
                    k.op("dve", lambda: V.tensor_tensor(out=Lm[:].rearrange("p (a b) -> p a b", b=8), in0=L[:, 4:36].rearrange("p (a b) -> p a b", b=8),
                                                        in1=gone[:].unsqueeze(2).to_broadcast([128, 4, 8]), op=ALU.add), reads=[B["L"], B["gone"]], writes=[B["Lm"]])
                    k.op("dve", lambda: V.tensor_reduce(out=rt[:, 4:5], in_=Lm[:], axis=AX.X, op=ALU.max), reads=[B["Lm"]], writes=R)
                    k.op("dve", lambda: V.tensor_scalar(out=one1[:], in0=Lm[:], scalar1=rt[:, 4:5], scalar2=None, op0=ALU.is_equal), reads=[B["Lm"]] + R, writes=[B["one1"]])
                    k.op("dve", lambda: V.tensor_scalar(out=Lm2[:], in0=one1[:], scalar1=-BIG, scalar2=None, op0=ALU.mult), reads=[B["one1"]], writes=[B["Lm2"]])
                    k.op("dve", lambda: V.tensor_tensor(out=Lm2[:], in0=Lm2[:], in1=Lm[:], op=ALU.add), reads=[B["Lm2"], B["Lm"]], writes=[B["Lm2"]])
                    k.op("dve", lambda: V.tensor_reduce(out=rt[:, 5:6], in_=Lm2[:], axis=AX.X, op=ALU.max), reads=[B["Lm2"]], writes=R)
                    k.op("dve", lambda: V.tensor_scalar(out=one2[:], in0=Lm2[:], scalar1=rt[:, 5:6], scalar2=None, op0=ALU.is_equal), reads=[B["Lm2"]] + R, writes=[B["one2"]])
                    k.op("dve", lambda: V.tensor_tensor(out=rt[:, 6:7], in0=rt[:, 4:5], in1=rt[:, 5:6], op=ALU.subtract), reads=R, writes=R)
                    k.op("act", lambda: S.activation(out=rt[:, 7:8], in_=rt[:, 6:7], func=AF.Sigmoid), reads=R, writes=R)
                    k.op("dve", lambda: V.tensor_tensor(out=rt[:, 8:9], in0=rt[:, 7:8], in1=rt[:, 3:4], op=ALU.mult), reads=R, writes=R)
                    k.op("dve", lambda: V.tensor_tensor(out=rt[:, 9:10], in0=rt[:, 3:4], in1=rt[:, 8:9], op=ALU.subtract), reads=R, writes=R)
                    k.op("dve", lambda: V.tensor_tensor(out=oh_bf[:], in0=one1[:], in1=one2[:], op=ALU.add), reads=[B["one1"], B["one2"]], writes=[B["oh"]])
                    k.op("pe", lambda: P.matmul(ps_rk, ustrict_bf[:], oh_bf[:], start=True, stop=False), reads=[B["oh"]] + CW, writes=[B["psrk"]], inc=False)
                    k.op("pe", lambda: P.matmul(ps_rk, ones_bf[:], cum_bf[:], start=False, stop=True), reads=[B["cum"]] + CW, writes=[B["psrk"]])
                    k.op("dve", lambda: V.tensor_copy(out=rk[:], in_=ps_rk), reads=[B["psrk"]], writes=[B["rk"]])
                    k.op("dve", lambda: V.tensor_tensor(out=cum_bf[:], in0=cum_bf[:], in1=oh_bf[:], op=ALU.add), reads=[B["cum"], B["oh"]], writes=[B["cum"]])
                    for kk_, oneX in ((0, one1), (1, one2)):
                        Bo = B["one1"] if kk_ == 0 else B["one2"]
                        c0 = 10 + 3 * kk_
                        k.op("dve", lambda oneX=oneX: V.tensor_tensor(out=tmp32[:], in0=oneX[:], in1=rk[:], op=ALU.mult), reads=[Bo, B["rk"]], writes=[B["tmp32"]])
                        k.op("dve", lambda c0=c0: V.tensor_reduce(out=rt[:, c0:c0 + 1], in_=tmp32[:], axis=AX.X, op=ALU.add), reads=[B["tmp32"]], writes=R)
                        k.op("dve", lambda oneX=oneX: V.tensor_tensor(out=tmp32[:], in0=oneX[:], in1=eoff[:], op=ALU.mult), reads=[Bo] + CW, writes=[B["tmp32"]])
                        k.op("dve", lambda c0=c0: V.tensor_reduce(out=rt[:, c0 + 1:c0 + 2], in_=tmp32[:], axis=AX.X, op=ALU.add), reads=[B["tmp32"]], writes=R)
                        k.op("dve", lambda c0=c0: V.tensor_scalar(out=rt[:, c0 + 2:c0 + 3], in0=rt[:, c0:c0 + 1], scalar1=float(CAP) - 0.5, scalar2=None, op0=ALU.is_lt), reads=R, writes=R)
                        k.op("dve", lambda c0=c0, kk_=kk_: V.tensor_tensor(out=w_all[:, 2 * Tg + kk_:2 * Tg + kk_ + 1], in0=rt[:, 8 + kk_:9 + kk_], in1=rt[:, c0 + 2:c0 + 3], op=ALU.mult),
                             reads=R, writes=[B_wall[Tg]])
                        k.op("dve", lambda c0=c0: V.tensor_scalar(out=rt[:, c0 + 2:c0 + 3], in0=rt[:, c0 + 2:c0 + 3], scalar1=-1.0e6, scalar2=1.0e6, op0=ALU.mult, op1=ALU.add), reads=R, writes=R)
                        k.op("dve", lambda c0=c0: V.tensor_tensor(out=rt[:, c0:c0 + 1], in0=rt[:, c0:c0 + 1], in1=rt[:, c0 + 1:c0 + 2], op=ALU.add), reads=R, writes=R)
                        k.op("dve", lambda c0=c0, kk_=kk_: V.tensor_tensor(out=slotf[:, kk_:kk_ + 1], in0=rt[:, c0:c0 + 1], in1=rt[:, c0 + 2:c0 + 3], op=ALU.add), reads=R, writes=[B["slotf"]])
                    k.op("dve", lambda: V.tensor_copy(out=slot_all[:, 2 * Tg:2 * Tg + 2], in_=slotf[:]), reads=[B["slotf"]], writes=[B_slot[Tg]])
                    for kk_ in range(2):
                        k.dma("pool", f"sc{s_}", lambda kk_=kk_: G.indirect_dma_start(out=xslots, out_offset=bass.IndirectOffsetOnAxis(ap=slot_all[:, 2 * Tg + kk_:2 * Tg + kk_ + 1], axis=0),
                                                                                     in_=hn_bf[s_][:, :], in_offset=None, bounds_check=bc_reg, oob_is_err=False),
                              reads=[B_hn[s_], B_slot[Tg]])

                stage12(0)
                for Tg in range(NT_A):
                    mid(Tg)
                    if Tg + 1 < NT_A:
                        stage12(Tg + 1)
                    routing(Tg)
            except _StopTile:
                pass
            k.barrier()

        with ExitStack() as es:
          if "B" in phases:
              wg_bf = [sbt(es, f"wg_bf{i}", [128, 8, DE], BF16) for i in range(2)]
              wu_bf = [sbt(es, f"wu_bf{i}", [128, 8, DE], BF16) for i in range(2)]
              wd_bf = [sbt(es, f"wd_bf{i}", [128, 4, D], BF16) for i in range(2)]
              xb = [sbt(es, f"xb{i}", [128, CT, D], BF16) for i in range(2)]
              xbT = sbt(es, "xbT", [128, 8, CAP], BF16)
              sg = [sbt(es, f"sg{i}", [128, 320], F32) for i in range(2)]
              hdnT = sbt(es, "hdnT", [128, 4, CAP], BF16)
              yt = [sbt(es, f"yt{i}", [128, D], F32) for i in range(2)]
              identb = sbt(es, "identb", [128, 128], BF16)
              pb_tp = [pst(es, f"pb_tp{i}", [128, D], BF16) for i in range(2)]
              pb_g = [pst(es, f"pb_g{i}", [128, 512], F32) for i in range(2)]
              pb_u = [pst(es, f"pb_u{i}", [128, 512], F32) for i in range(2)]
              pb_y = [pst(es, f"pb_y{i}", [128, 512], F32) for i in range(2)]
              Bw = [Buf("w0"), Buf("w1")]
              Bxb = [Buf("xb0"), Buf("xb1")]
              BxbT, BhdnT, Bid = Buf("xbT"), Buf("hdnT"), Buf("identb")
              Bsg = [Buf("sg0"), Buf("sg1")]
              Byt = [Buf("yt0"), Buf("yt1")]
              Bptp = [Buf("ptp0"), Buf("ptp1")]
              Bpg = [Buf("pg0"), Buf("pg1")]
              Bpu = [Buf("pu0"), Buf("pu1")]
              Bpy = [Buf("py0"), Buf("py1")]
              for s_ in ("ew0", "ew1", "xb0", "xb1", "ys0", "ys1", "idb"):
                  k.newsem(s_)
              k.dma("pool", "idb", lambda: G.dma_start(out=identb[:], in_=c_ident), writes=[Bid])

              def load_expert(e):
                  s_ = e % 2
                  k.dma("pool", f"ew{s_}", lambda: G.dma_start(out=wg_bf[s_][:], in_=w_eg[e].rearrange("(kc p) n -> p kc n", p=128)), writes=[Bw[s_]])
                  k.dma("pool", f"ew{s_}", lambda: G.dma_start(out=wu_bf[s_][:], in_=w_eu[e].rearrange("(kc p) n -> p kc n", p=128)))
                  k.dma("pool", f"ew{s_}", lambda: G.dma_start(out=wd_bf[s_][:], in_=w_ed[e].rearrange("(kc p) n -> p kc n", p=128)))
                  Bw[s_].w = (f"ew{s_}", k.cnt[f"ew{s_}"])
                  k.dma("sp", f"xb{s_}", lambda: T.dma_start(out=xb[s_][:], in_=xslots[e * CAP:(e + 1) * CAP, :].rearrange("(st p) n -> p st n", p=128)), writes=[Bxb[s_]])

              load_expert(0)
              ycount = 0
              for e in range(NE):
                  s_ = e % 2
                  if e + 1 < NE:
                      load_expert(e + 1)
                  for st in range(CT):
                      tp, Btp = pb_tp[st % 2], Bptp[st % 2]
                      for kc in range(8):
                          k.op("pe", lambda st=st, kc=kc, tp=tp: P.transpose(tp[:, kc * 128:(kc + 1) * 128], xb[s_][:, st, kc * 128:(kc + 1) * 128], identb[:]),
                               reads=[Bxb[s_], Bid], writes=[Btp], inc=(kc == 7))
                      eng = "act" if st % 2 == 0 else "dve"
                      if eng == "act":
                          k.op("act", lambda st=st, tp=tp: S.copy(out=xbT[:, :, st * 128:(st + 1) * 128], in_=tp[:].rearrange("p (a b) -> p a b", b=128)), reads=[Btp], writes=[BxbT])
                      else:
                          k.op("dve", lambda st=st, tp=tp: V.tensor_copy(out=xbT[:, :, st * 128:(st + 1) * 128], in_=tp[:].rearrange("p (a b) -> p a b", b=128)), reads=[Btp], writes=[BxbT])
                  i2 = 0
                  for fc in range(4):
                      for hf in range(2):
                          pg, pu = pb_g[i2 % 2], pb_u[i2 % 2]
                          Bg_, Bu_ = Bpg[i2 % 2], Bpu[i2 % 2]
                          sgi, Bsgi = sg[i2 % 2], Bsg[i2 % 2]
                          i2 += 1
                          for kc in range(8):
                              k.op("pe", lambda kc=kc, fc=fc, hf=hf, pg=pg: P.matmul(pg[:, 0:320], wg_bf[s_][:, kc, fc * 128:(fc + 1) * 128], xbT[:, kc, hf * 320:(hf + 1) * 320], start=(kc == 0), stop=(kc == 7)),
                                   reads=[Bw[s_], BxbT], writes=[Bg_], inc=(kc == 7))
                          for kc in range(8):
                              k.op("pe", lambda kc=kc, fc=fc, hf=hf, pu=pu: P.matmul(pu[:, 0:320], wu_bf[s_][:, kc, fc * 128:(fc + 1) * 128], xbT[:, kc, hf * 320:(hf + 1) * 320], start=(kc == 0), stop=(kc == 7)),
                                   reads=[Bw[s_], BxbT], writes=[Bu_], inc=(kc == 7))
                          k.op("act", lambda pg=pg, sgi=sgi: S.activation(out=sgi[:], in_=pg[:, 0:320], func=AF.Silu), reads=[Bg_], writes=[Bsgi])
                          k.op("dve", lambda pu=pu, sgi=sgi, fc=fc, hf=hf: V.tensor_tensor(out=hdnT[:, fc, hf * 320:(hf + 1) * 320], in0=pu[:, 0:320], in1=sgi[:], op=ALU.mult),
                               reads=[Bsgi, Bu_], writes=[BhdnT])
                  for st in range(CT):
                      ys = ycount % 2
                      ycount += 1
                      for nh in range(2):
                          py, Bpy_ = pb_y[nh], Bpy[nh]
                          for fc in range(4):
                              k.op("pe", lambda fc=fc, st=st, nh=nh, py=py: P.matmul(py[:], hdnT[:, fc, st * 128:(st + 1) * 128], wd_bf[s_][:, fc, nh * 512:(nh + 1) * 512], start=(fc == 0), stop=(fc == 3)),
                                   reads=[BhdnT, Bw[s_]], writes=[Bpy_], inc=(fc == 3))
                          if nh == 0:
                              k.op("act", lambda py=py, ys=ys: S.copy(out=yt[ys][:, 0:512], in_=py[:]), reads=[Bpy_], writes=[Byt[ys]])
                          else:
                              k.op("dve", lambda py=py, ys=ys: V.tensor_copy(out=yt[ys][:, 512:1024], in_=py[:]), reads=[Bpy_], writes=[Byt[ys]])
                      r0 = e * CAP + st * 128
                      k.dma("sp", f"ys{ys}", lambda ys=ys, r0=r0: T.dma_start(out=ybuf[r0:r0 + 128, :], in_=yt[ys][:]), reads=[Byt[ys]])
              k.barrier()

        with ExitStack() as es:
          if "C" in phases:
              y1 = [sbt(es, f"y1_{i}", [128, D], F32) for i in range(2)]
              y2 = [sbt(es, f"y2_{i}", [128, D], F32) for i in range(2)]
              xr = [sbt(es, f"xr{i}", [128, D], F32) for i in range(2)]
              By1 = [Buf("y1_0"), Buf("y1_1")]
              By2 = [Buf("y2_0"), Buf("y2_1")]
              Bxr = [Buf("xr0"), Buf("xr1")]
              for s_ in ("ga0", "ga1", "gb0", "gb1", "xr0", "xr1", "os0", "os1"):
                  k.newsem(s_)
              for i in range(2):
                  k.op("dve", lambda i=i: V.memset(y1[i][:], 0.0), writes=[By1[i]])
                  k.op("dve", lambda i=i: V.memset(y2[i][:], 0.0), writes=[By2[i]])

              def load_c(Tg):
                  s_ = Tg % 2
                  k.dma("sp", f"xr{s_}", lambda: T.dma_start(out=xr[s_][:], in_=out[Tg * 128:(Tg + 1) * 128, :]), writes=[Bxr[s_]])
                  k.dma("pool", f"ga{s_}", lambda: G.indirect_dma_start(out=y1[s_][:, :], out_offset=None, in_=ybuf, in_offset=bass.IndirectOffsetOnAxis(ap=slot_all[:, 2 * Tg:2 * Tg + 1], axis=0),
                                                                        bounds_check=bc_reg, oob_is_err=False), reads=[B_slot[Tg]], writes=[By1[s_]])
                  k.dma("pool", f"gb{s_}", lambda: G.indirect_dma_start(out=y2[s_][:, :], out_offset=None, in_=ybuf, in_offset=bass.IndirectOffsetOnAxis(ap=slot_all[:, 2 * Tg + 1:2 * Tg + 2], axis=0),
                                                                        bounds_check=bc_reg, oob_is_err=False), reads=[B_slot[Tg]], writes=[By2[s_]])

              load_c(0)
              for Tg in range(NT):
                  s_ = Tg % 2
                  if Tg + 1 < NT:
                      load_c(Tg + 1)
                  k.op("dve", lambda: V.tensor_scalar(out=y1[s_][:], in0=y1[s_][:], scalar1=w_all[:, 2 * Tg:2 * Tg + 1], scalar2=None, op0=ALU.mult), reads=[By1[s_], B_wall[Tg]], writes=[By1[s_]])
                  k.op("dve", lambda: V.tensor_tensor(out=xr[s_][:], in0=xr[s_][:], in1=y1[s_][:], op=ALU.add), reads=[By1[s_], Bxr[s_]], writes=[Bxr[s_]])
                  k.op("dve", lambda: V.tensor_scalar(out=y2[s_][:], in0=y2[s_][:], scalar1=w_all[:, 2 * Tg + 1:2 * Tg + 2], scalar2=None, op0=ALU.mult), reads=[By2[s_], B_wall[Tg]], writes=[By2[s_]])
                  k.op("dve", lambda: V.tensor_tensor(out=xr[s_][:], in0=xr[s_][:], in1=y2[s_][:], op=ALU.add), reads=[By2[s_], Bxr[s_]], writes=[Bxr[s_]])
                  k.dma("sp", f"os{s_}", lambda: T.dma_start(out=out[Tg * 128:(Tg + 1) * 128, :], in_=xr[s_][:]), reads=[Bxr[s_]])
              k.barrier()
    k.close()
    return nc


def _consts():
    wins = (2, 4, 8, 16)
    A = np.zeros((12, 128, 128), np.float32)
    for g, w in enumerate(wins):
        for t in range(128):
            cnt = min(t + 1, w)
            for j in range(max(0, t - w + 1), t + 1):
                A[g, j, t] += 1.0 / cnt
            A[g, t, t] -= 1.0
            for j in range(t - w + 1, t + 1):
                if j >= 0:
                    A[4 + g, j, t] += 1.0 / w
                else:
                    A[8 + g, 128 + j, t] += 1.0 / w
            A[4 + g, t, t] -= 1.0
    slopes = [2.0 ** (-8.0 * (h + 1) / 4) for h in range(4)]
    al = np.zeros((128, 64), np.float32)
    kl = np.arange(128, dtype=np.float32)
    for h in range(4):
        for d in range(16):
            al[:, h * 16 + d] = slopes[h] * (kl - 127.0 - 128.0 * d)
    kk = np.arange(128)[:, None]
    qq = np.arange(128)[None, :]
    cmask = (kk <= qq).astype(np.float32)
    ustrict = (kk < qq).astype(np.float32)
    eoff = np.tile((np.arange(NE, dtype=np.float32) * CAP)[None, :], (128, 1))
    ident = np.eye(128, dtype=np.float32)
    return {"c_poolA": A, "c_alibi": al, "c_cmask": cmask, "c_ustrict": ustrict, "c_eoff": eoff, "c_ident": ident}


_NC = None


def kernel(**inputs):
    global _NC
    if _NC is None:
        _NC = build_program()
    f = lambda a: np.ascontiguousarray(np.asarray(a, dtype=np.float32))
    x = f(inputs["x"])
    shared = {
        "norm1_g": f(inputs["norm1_g"]).reshape(1, D),
        "w_in": f(inputs["w_in"]).reshape(D, IN_W),
        "pool_w": f(inputs["pool_w"]).reshape(4, 128, 128),
        "pool_scale": f(inputs["pool_scale"]).reshape(1, 512),
        "w_pool_up": f(inputs["w_pool_up"]).reshape(512, D),
        "q_norm_g": f(inputs["q_norm_g"]).reshape(1, 64),
        "k_norm_g": f(inputs["k_norm_g"]).reshape(1, 64),
        "lambda_q1": f(inputs["lambda_q1"]).reshape(1, 64),
        "lambda_k1": f(inputs["lambda_k1"]).reshape(1, 64),
        "lambda_q2": f(inputs["lambda_q2"]).reshape(1, 64),
        "lambda_k2": f(inputs["lambda_k2"]).reshape(1, 64),
        "subln_g": f(inputs["subln_g"]).reshape(1, 128),
        "w_attn_up": f(inputs["w_attn_up"]).reshape(512, D),
        "w_out": f(inputs["w_out"]).reshape(D, D),
        "norm2_g": f(inputs["norm2_g"]).reshape(1, D),
        "w_router_group": f(inputs["w_router_group"]).reshape(D, 4),
        "b_router_group": f(inputs["b_router_group"]).reshape(1, 4),
        "w_router_expert": f(inputs["w_router_expert"]).reshape(D, NE),
        "b_router_expert": f(inputs["b_router_expert"]).reshape(1, NE),
        "w_expert_gate": f(inputs["w_expert_gate"]).reshape(NE, D, DE),
        "w_expert_up": f(inputs["w_expert_up"]).reshape(NE, D, DE),
        "w_expert_down": f(inputs["w_expert_down"]).reshape(NE, DE, D),
    }
    shared.update(_consts())
    in_maps = []
    for c in range(N_CORES):
        m = dict(shared)
        m["x"] = np.ascontiguousarray(x[c * SEQ_PER_CORE:(c + 1) * SEQ_PER_CORE].reshape(NTOK, D))
        in_maps.append(m)
    res = run_bass_kernel_spmd(_NC, in_maps, core_ids=list(range(N_CORES)))
    outs = [np.asarray(r["out"], dtype=np.float32).reshape(SEQ_PER_CORE, SEQ, D) for r in res.results]
    return np.concatenate(outs, axis=0)
```

```python
import math
from contextlib import ExitStack
import numpy as np
import ml_dtypes
import concourse.bass as bass
import concourse.mybir as mybir
from concourse.bass_utils import run_bass_kernel_spmd

F32 = mybir.dt.float32
BF16 = mybir.dt.bfloat16
I32 = mybir.dt.int32
ALU = mybir.AluOpType
AF = mybir.ActivationFunctionType
AX = mybir.AxisListType

N_CORES = 8
D = 1024
SEQ = 2048
SEQ_PER_CORE = 4
TPS = SEQ // 128
NT = SEQ_PER_CORE * TPS
NTOK = NT * 128
IN_W = 4096
NE = 32
DE = 512
CAP = 640
CT = CAP // 128
NSLOT = NE * CAP
EPS = 1e-6
LAM_INIT = 0.8 - 0.6 * math.exp(0.0)
BIG = 1.0e4
LOOKAHEAD = 3


class Buf:
    __slots__ = ("name", "w", "r")

    def __init__(self, name):
        self.name = name
        self.w = None
        self.r = {}


class K:
    def __init__(self, nc):
        self.nc = nc
        self.eng = {"pe": nc.tensor, "act": nc.scalar, "dve": nc.vector, "pool": nc.gpsimd, "sp": nc.sync}
        self.sems = {}
        self.cnt = {}
        self.waited = {}
        self._stack = []
        self.pool_fifo = []
        for e in ("pe", "act", "dve", "pool"):
            self.newsem(e)

    def newsem(self, key):
        cm = self.nc.semaphore("s_" + key)
        h = cm.__enter__()
        self._stack.append(cm)
        self.sems[key] = h
        self.cnt[key] = 0
        return key

    def close(self):
        for cm in reversed(self._stack):
            cm.__exit__(None, None, None)

    def _wait(self, e, key, val):
        if self.waited.get((e, key), 0) >= val:
            return
        self.waited[(e, key)] = val
        self.eng[e].wait_ge(self.sems[key], val)

    def _deps(self, e, reads, writes):
        deps = {}

        def add(tok, skip_same):
            if tok is None:
                return
            kk, v = tok
            if kk == e and skip_same:
                return
            if deps.get(kk, 0) < v:
                deps[kk] = v
        for b in reads:
            add(b.w, e == "pe")
        for b in writes:
            add(b.w, e == "pe")
            for kk, v in b.r.items():
                add((kk, v), e == "pe")
        for kk, v in deps.items():
            self._wait(e, kk, v)

    def _record(self, tok, reads, writes):
        for b in reads:
            if b.r.get(tok[0], 0) < tok[1]:
                b.r[tok[0]] = tok[1]
        for b in writes:
            b.w = tok
            b.r = {}

    def op(self, e, ins_fn, reads=(), writes=(), inc=True):
        self._deps(e, reads, writes)
        ins = ins_fn()
        if inc:
            self.cnt[e] += 1
            ins.then_inc(self.sems[e], 1)
            tok = (e, self.cnt[e])
        else:
            tok = (e, self.cnt[e] + 1)
        self._record(tok, reads, writes)
        return ins

    def dma(self, q, semkey, ins_fn, reads=(), writes=()):
        self._deps(q, reads, writes)
        ins = ins_fn()
        self.cnt[semkey] += 16
        ins.then_inc(self.sems[semkey], 16)
        self._record((semkey, self.cnt[semkey]), reads, writes)
        return ins

    def barrier(self, engines=("pe", "act", "dve", "pool", "sp")):
        for e in engines:
            for kk, v in self.cnt.items():
                if v > 0 and kk != e:
                    self._wait(e, kk, v)


class _StopTile(Exception):
    pass


def build_program(n_seq=SEQ_PER_CORE, phases="ABC", nt_lim=None, stop_at=None):
    nc = bass.Bass("TRN2", target_bir_lowering=False)
    NT = n_seq * TPS
    NTOK = NT * 128
    NT_A = NT if nt_lim is None else nt_lim

    def din(name, shape, dt=F32):
        return nc.dram_tensor(name, list(shape), dt, kind="ExternalInput").ap()

    x = din("x", [NTOK, D])
    norm1_g = din("norm1_g", [1, D])
    w_in = din("w_in", [D, IN_W])
    pool_w = din("pool_w", [4, 128, 128])
    pool_scale = din("pool_scale", [1, 512])
    w_pool_up = din("w_pool_up", [512, D])
    q_norm_g = din("q_norm_g", [1, 64])
    k_norm_g = din("k_norm_g", [1, 64])
    lq1 = din("lambda_q1", [1, 64])
    lk1 = din("lambda_k1", [1, 64])
    lq2 = din("lambda_q2", [1, 64])
    lk2 = din("lambda_k2", [1, 64])
    subln_g = din("subln_g", [1, 128])
    w_attn_up = din("w_attn_up", [512, D])
    w_out = din("w_out", [D, D])
    norm2_g = din("norm2_g", [1, D])
    w_rg = din("w_router_group", [D, 4])
    b_rg = din("b_router_group", [1, 4])
    w_re = din("w_router_expert", [D, NE])
    b_re = din("b_router_expert", [1, NE])
    NE_decl = NE if "B" in phases else 1
    w_eg = din("w_expert_gate", [NE_decl, D, DE])
    w_eu = din("w_expert_up", [NE_decl, D, DE])
    w_ed = din("w_expert_down", [NE_decl, DE, D])
    c_poolA = din("c_poolA", [12, 128, 128])
    c_alibi = din("c_alibi", [128, 64])
    c_cmask = din("c_cmask", [128, 128])
    c_ustrict = din("c_ustrict", [128, 128])
    c_eoff = din("c_eoff", [128, NE])
    c_ident = din("c_ident", [128, 128])

    out = nc.dram_tensor("out", [NTOK, D], F32, kind="ExternalOutput").ap()
    xslots = nc.dram_tensor("xslots", [NSLOT, D], BF16, kind="Internal").ap()
    ybuf = nc.dram_tensor("ybuf", [NSLOT, D], F32, kind="Internal").ap()

    k = K(nc)
    V, S, P, G, T = nc.vector, nc.scalar, nc.tensor, nc.gpsimd, nc.sync
    bc_reg = G.to_reg(NSLOT - 1)

    with ExitStack() as top:
        def sbt(es, name, shape, dt):
            return es.enter_context(nc.sbuf_tensor(name, list(shape), dt))

        def pst(es, name, shape, dt):
            return es.enter_context(nc.psum_tensor(name, list(shape), dt))

        slot_all = sbt(top, "slot_all", [128, NT * 2], I32)
        w_all = sbt(top, "w_all", [128, NT * 2], F32)
        B_slot = [Buf(f"slot{i}") for i in range(NT)]
        B_wall = [Buf(f"wall{i}") for i in range(NT)]

        with ExitStack() as es:
            zt = sbt(es, "zt", [128, 8 * D], BF16)
            Bz = Buf("zt")
            k.newsem("zf")
            k.op("dve", lambda: V.memset(zt[:], 0.0), writes=[Bz])
            for r0 in range(0, NSLOT, 1024):
                k.dma("sp", "zf", lambda r0=r0: T.dma_start(out=xslots[r0:r0 + 1024, :].rearrange("(p a) n -> p (a n)", p=128), in_=zt[:]), reads=[Bz])
            k.barrier()

        with ExitStack() as es:
            w_in_bf = sbt(es, "w_in_bf", [128, 8, IN_W], BF16)
            w_out_bf = sbt(es, "w_out_bf", [128, 8, D], BF16)
            wpu_bf = sbt(es, "wpu_bf", [128, 4, D], BF16)
            wau_bf = sbt(es, "wau_bf", [128, 4, D], BF16)
            poolw_bf = sbt(es, "poolw_bf", [128, 4, 128], BF16)
            wr_f = sbt(es, "wr_f", [128, 8, 36], F32)
            g1_bc = sbt(es, "g1_bc", [128, D], F32)
            g2_bc = sbt(es, "g2_bc", [128, D], F32)
            gsub_bc = sbt(es, "gsub_bc", [128, 128], F32)
            gq_bc = sbt(es, "gq_bc", [128, 64], F32)
            gk_bc = sbt(es, "gk_bc", [128, 64], F32)
            lam_in = sbt(es, "lam_in", [128, 4, 64], F32)
            lam_w = sbt(es, "lam_w", [128, 8], F32)
            g2col = sbt(es, "g2col", [128, 8], F32)
            g2rows = sbt(es, "g2rows", [8, 128], F32)
            psrows = sbt(es, "psrows", [4, 128], F32)
            pscale = sbt(es, "pscale", [128, 4], F32)
            rbias = sbt(es, "rbias", [128, 36], F32)
            poolA_bf = sbt(es, "poolA_bf", [128, 12, 128], BF16)
            alibi = sbt(es, "alibi", [128, 64], F32)
            cmask_bf = sbt(es, "cmask_bf", [128, 128], BF16)
            ustrict_bf = sbt(es, "ustrict_bf", [128, 128], BF16)
            ones_bf = sbt(es, "ones_bf", [128, 128], BF16)
            eoff = sbt(es, "eoff", [128, NE], F32)
            ident_f = sbt(es, "ident_f", [128, 128], F32)
            ident_bf = sbt(es, "ident_bf", [128, 128], BF16)
            kT = sbt(es, "kT", [128, 4, SEQ], BF16)
            v_aug = sbt(es, "v_aug", [128, TPS, 4, 130], BF16)
            cum_bf = sbt(es, "cum_bf", [128, NE], BF16)
            xt = [sbt(es, f"xt{i}", [128, D], F32) for i in range(2)]
            eps_col = sbt(es, "eps_col", [128, 1], F32)
            st1 = sbt(es, "st1", [128, 8], F32)
            h_bf = sbt(es, "h_bf", [128, D], BF16)
            hT = sbt(es, "hT", [128, 8, 128], BF16)
            u_bf = [sbt(es, f"u_bf{i}", [128, 512], BF16) for i in range(2)]
            qkst = sbt(es, "qkst", [128, 16], F32)
            qn_bf = sbt(es, "qn_bf", [128, 512], BF16)
            kn_bf = sbt(es, "kn_bf", [128, 512], BF16)
            qT = sbt(es, "qT", [128, 4, 128], BF16)
            gp_sb = sbt(es, "gp_sb", [128, D], F32)
            ga_sb = sbt(es, "ga_sb", [128, D], F32)
            dT_bf = sbt(es, "dT_bf", [128, 512], BF16)
            yT_bf = sbt(es, "yT_bf", [128, 512], BF16)
            NPT = 8
            PT = [sbt(es, f"PT{i}", [128, 128], BF16) for i in range(NPT)]
            o_all = sbt(es, "o_all", [128, 4, 128], F32)
            ost = sbt(es, "ost", [128, 16], F32)
            otmp = sbt(es, "otmp", [128, 128], F32)
            o_n = sbt(es, "o_n", [128, 512], BF16)
            o_nT = sbt(es, "o_nT", [128, 4, 128], BF16)
            t1 = sbt(es, "t1", [128, D], F32)
            t2 = sbt(es, "t2", [128, D], F32)
            mix_bf = sbt(es, "mix_bf", [128, D], BF16)
            junk = mix_bf
            sq = t1[:, 0:512]
            qtmp = t2[:, 0:512]
            mixT = sbt(es, "mixT", [128, 8, 128], BF16)
            x1 = [sbt(es, "x1_0", [128, D], F32)] * 2
            hn_bf = [sbt(es, f"hn_bf{i}", [128, D], BF16) for i in range(2)]
            x1T = sbt(es, "x1T", [128, 4, 128], F32)
            L = sbt(es, "L", [128, 36], F32)
            rt = sbt(es, "rt", [128, 16], F32)
            gone = sbt(es, "gone", [128, 4], F32)
            gexp = sbt(es, "gexp", [128, 4], F32)
            Lm = sbt(es, "Lm", [128, NE], F32)
            Lm2 = sbt(es, "Lm2", [128, NE], F32)
            one1 = sbt(es, "one1", [128, NE], F32)
            one2 = sbt(es, "one2", [128, NE], F32)
            oh_bf = sbt(es, "oh_bf", [128, NE], BF16)
            rk = sbt(es, "rk", [128, NE], F32)
            tmp32 = sbt(es, "tmp32", [128, NE], F32)
            slotf = sbt(es, "slotf", [128, 2], F32)

            ps_tp = pst(es, "ps_tp", [128, D], BF16)
            ps_mm = [pst(es, f"ps_mm{i}", [128, 512], F32) for i in range(2)]
            ps_sc = [pst(es, f"ps_sc{i}", [128, 512], F32) for i in range(2)]
            ps_o = [pst(es, f"ps_o{i}", [128, 512], F32) for i in range(2)]
            ps_misc = pst(es, "ps_misc", [128, 512], F32)
            ps_tpf = ps_misc

            names = ["otmp", "wts", "wts2", "junk", "st1", "h", "pstp", "hT", "sq", "qtmp", "qkst", "qn", "kn", "qT", "kT", "vaug",
                     "gp", "ga", "dT", "yT", "pstpf", "pssc", "psmisc", "oall", "ost", "on", "onT", "t1", "t2", "mix",
                     "mixT", "x1T", "L", "rt", "gone", "gexp", "Lm", "Lm2", "one1", "one2", "oh", "rk", "tmp32",
                     "slotf", "cum", "pslg", "psrk", "consts"]
            B = {n: Buf(n) for n in names}
            B_xt = [Buf("xt0"), Buf("xt1")]
            B_u = [Buf("u0"), Buf("u1")]
            B_x1 = [Buf("x1_0")] * 2
            B_hn = [Buf("hn0"), Buf("hn1")]
            B_mm = [Buf("mm0"), Buf("mm1")]
            B_PT = [Buf(f"PT{i}") for i in range(NPT)]
            B_sc = [Buf(f"sc{i}") for i in range(2)]
            B_pso = [Buf("pso0"), Buf("pso1")]
            for s_ in ("wlp", "wls", "xl0", "xl1", "xs0", "xs1", "sc0", "sc1"):
                k.newsem(s_)

            W = ()
            Cst = [B["consts"]]
            for q4 in range(4):
                k.dma("pool", "wlp", lambda q4=q4: G.dma_start(out=w_in_bf[:, :, q4 * 1024:(q4 + 1) * 1024], in_=w_in[:, q4 * 1024:(q4 + 1) * 1024].rearrange("(kc p) n -> p kc n", p=128)), writes=W)
            k.dma("pool", "wlp", lambda: G.dma_start(out=w_out_bf[:], in_=w_out.rearrange("(kc p) n -> p kc n", p=128)), writes=W)
            k.dma("pool", "wlp", lambda: G.dma_start(out=wpu_bf[:], in_=w_pool_up.rearrange("(kc p) n -> p kc n", p=128)), writes=W)
            k.dma("pool", "wlp", lambda: G.dma_start(out=wau_bf[:], in_=w_attn_up.rearrange("(kc p) n -> p kc n", p=128)), writes=W)
            k.dma("pool", "wlp", lambda: G.dma_start(out=poolw_bf[:], in_=pool_w.rearrange("g c d -> c g d")), writes=W)
            k.dma("pool", "wlp", lambda: G.dma_start(out=poolA_bf[:], in_=c_poolA.rearrange("a j t -> j a t")), writes=W)
            k.dma("pool", "wlp", lambda: G.dma_start(out=cmask_bf[:], in_=c_cmask), writes=W)
            k.dma("pool", "wlp", lambda: G.dma_start(out=ustrict_bf[:], in_=c_ustrict), writes=W)
            k.dma("pool", "wlp", lambda: G.dma_start(out=ident_bf[:], in_=c_ident), writes=W)
            k.dma("sp", "wls", lambda: T.dma_start(out=ident_f[:], in_=c_ident), writes=W)
            k.dma("sp", "wls", lambda: T.dma_start(out=alibi[:], in_=c_alibi), writes=W)
            k.dma("sp", "wls", lambda: T.dma_start(out=eoff[:], in_=c_eoff), writes=W)
            k.dma("sp", "wls", lambda: T.dma_start(out=wr_f[:, :, 0:4], in_=w_rg.rearrange("(kc p) n -> p kc n", p=128)), writes=W)
            k.dma("sp", "wls", lambda: T.dma_start(out=wr_f[:, :, 4:36], in_=w_re.rearrange("(kc p) n -> p kc n", p=128)), writes=W)
            k.dma("sp", "wls", lambda: T.dma_start(out=g1_bc[:], in_=norm1_g[0:1, :].to_broadcast([128, D])), writes=W)
            k.dma("sp", "wls", lambda: T.dma_start(out=g2_bc[:], in_=norm2_g[0:1, :].to_broadcast([128, D])), writes=W)
            k.dma("sp", "wls", lambda: T.dma_start(out=gsub_bc[:], in_=subln_g[0:1, :].to_broadcast([128, 128])), writes=W)
            k.dma("sp", "wls", lambda: T.dma_start(out=gq_bc[:], in_=q_norm_g[0:1, :].to_broadcast([128, 64])), writes=W)
            k.dma("sp", "wls", lambda: T.dma_start(out=gk_bc[:], in_=k_norm_g[0:1, :].to_broadcast([128, 64])), writes=W)
            for i, lv in enumerate((lq1, lk1, lq2, lk2)):
                k.dma("sp", "wls", lambda i=i, lv=lv: T.dma_start(out=lam_in[:, i, :], in_=lv[0:1, :].to_broadcast([128, 64])), writes=W)
            k.dma("sp", "wls", lambda: T.dma_start(out=g2rows[:], in_=norm2_g.rearrange("o (kc p) -> (o kc) p", p=128)), writes=W)
            k.dma("sp", "wls", lambda: T.dma_start(out=psrows[:], in_=pool_scale.rearrange("o (g p) -> (o g) p", p=128)), writes=W)
            k.dma("sp", "wls", lambda: T.dma_start(out=rbias[:, 0:4], in_=b_rg[0:1, :].to_broadcast([128, 4])), writes=W)
            k.dma("sp", "wls", lambda: T.dma_start(out=rbias[:, 4:36], in_=b_re[0:1, :].to_broadcast([128, NE])), writes=W)
            B["wts"].w = ("wlp", k.cnt["wlp"])
            B["wts2"].w = ("wls", k.cnt["wls"])
            W = [B["wts"], B["wts2"]]

            k.op("dve", lambda: V.memset(ones_bf[:], 1.0), writes=Cst)
            k.op("dve", lambda: V.memset(eps_col[:], EPS), writes=Cst)
            k.op("dve", lambda: V.memset(cum_bf[:], 0.0), writes=[B["cum"]])
            k.op("pool", lambda: G.memset(v_aug[:], 1.0), writes=[B["vaug"]])
            k.op("pool", lambda: G.memset(hn_bf[0][:], 0.0), writes=[B_hn[0]])
            k.op("pool", lambda: G.memset(hn_bf[1][:], 0.0), writes=[B_hn[1]])
            k.op("pe", lambda: P.transpose(ps_misc[:, 0:8], g2rows[:], ident_f[0:8, 0:8]), reads=W, writes=[B["psmisc"]])
            k.op("dve", lambda: V.tensor_copy(out=g2col[:], in_=ps_misc[:, 0:8]), reads=[B["psmisc"]], writes=Cst)
            k.op("pe", lambda: P.transpose(ps_misc[:, 0:4], psrows[:], ident_f[0:4, 0:4]), reads=W, writes=[B["psmisc"]])
            k.op("dve", lambda: V.tensor_copy(out=pscale[:], in_=ps_misc[:, 0:4]), reads=[B["psmisc"]], writes=Cst)
            for kc in range(8):
                k.op("dve", lambda kc=kc: V.tensor_scalar(out=wr_f[:, kc, :], in0=wr_f[:, kc, :], scalar1=g2col[:, kc:kc + 1],
                                                           scalar2=None, op0=ALU.mult), reads=W + Cst, writes=Cst)
            k.op("dve", lambda: V.tensor_tensor(out=gq_bc[:], in0=gq_bc[:], in1=gk_bc[:], op=ALU.mult), reads=W, writes=Cst)
            k.op("dve", lambda: V.tensor_scalar(out=gq_bc[:], in0=gq_bc[:], scalar1=0.125, scalar2=None, op0=ALU.mult), reads=Cst, writes=Cst)
            k.op("dve", lambda: V.tensor_scalar(out=gsub_bc[:], in0=gsub_bc[:], scalar1=1.0 - LAM_INIT, scalar2=None, op0=ALU.mult), reads=W, writes=Cst)
            k.op("dve", lambda: V.tensor_tensor(out=lam_in[:, 0, :], in0=lam_in[:, 0, :], in1=lam_in[:, 1, :], op=ALU.mult), reads=W, writes=Cst)
            k.op("dve", lambda: V.tensor_tensor(out=lam_in[:, 2, :], in0=lam_in[:, 2, :], in1=lam_in[:, 3, :], op=ALU.mult), reads=Cst, writes=Cst)
            k.op("dve", lambda: V.tensor_reduce(out=lam_w[:, 0:1], in_=lam_in[:, 0, :], axis=AX.X, op=ALU.add), reads=Cst, writes=Cst)
            k.op("dve", lambda: V.tensor_reduce(out=lam_w[:, 1:2], in_=lam_in[:, 2, :], axis=AX.X, op=ALU.add), reads=Cst, writes=Cst)
            k.op("act", lambda: S.activation(out=lam_w[:, 2:4], in_=lam_w[:, 0:2], func=AF.Exp), reads=Cst, writes=Cst)
            k.op("dve", lambda: V.tensor_tensor(out=lam_w[:, 4:5], in0=lam_w[:, 2:3], in1=lam_w[:, 3:4], op=ALU.subtract), reads=Cst, writes=Cst)
            k.op("dve", lambda: V.tensor_scalar(out=lam_w[:, 5:6], in0=lam_w[:, 4:5], scalar1=LAM_INIT, scalar2=-1.0, op0=ALU.add, op1=ALU.mult), reads=Cst, writes=Cst)
            nlam = lam_w[:, 5:6]
            CW = [B["wts"], B["wts2"], B["consts"]]

            def load_x(Tg):
                s_ = Tg % 2
                k.dma("sp", f"xl{s_}", lambda: T.dma_start(out=xt[s_][:], in_=x[Tg * 128:(Tg + 1) * 128, :]), writes=[B_xt[s_]])

            try:
                if stop_at == 0:
                    raise _StopTile
                load_x(0)
                for Tg in range(NT_A):
                    t = Tg % TPS
                    s_ = Tg % 2
                    if Tg + 1 < NT_A:
                        load_x(Tg + 1)
                    xs, Bx = xt[s_], B_xt[s_]
                    k.op("act", lambda: S.activation(out=junk[:], in_=xs[:], func=AF.Square, accum_out=st1[:, 0:1]), reads=[Bx], writes=[B["st1"], B["mix"]])
                    k.op("act", lambda: S.activation(out=st1[:, 1:2], in_=st1[:, 0:1], func=AF.Ln, scale=1.0 / D, bias=eps_col[:, 0:1]), reads=[B["st1"]], writes=[B["st1"]])
                    k.op("act", lambda: S.activation(out=st1[:, 1:2], in_=st1[:, 1:2], func=AF.Exp, scale=-0.5), reads=[B["st1"]], writes=[B["st1"]])
                    k.op("act", lambda: S.activation(out=t2[:], in_=xs[:], func=AF.Copy, scale=st1[:, 1:2]), reads=[Bx, B["st1"]], writes=[B["t2"]])
                    k.op("dve", lambda: V.tensor_tensor(out=h_bf[:], in0=t2[:], in1=g1_bc[:], op=ALU.mult), reads=[B["t2"]] + CW, writes=[B["h"]])
                    for kc in range(8):
                        k.op("pe", lambda kc=kc: P.transpose(ps_tp[:, kc * 128:(kc + 1) * 128], h_bf[:, kc * 128:(kc + 1) * 128], ident_bf[:]),
                             reads=[B["h"]] + CW, writes=[B["pstp"]], inc=(kc == 7))
                    k.op("act", lambda: S.copy(out=hT[:].rearrange("p a b -> p (a b)"), in_=ps_tp[:]), reads=[B["pstp"]], writes=[B["hT"]])
                    if stop_at == 1:
                        raise _StopTile
                    for cb in range(8):
                        mm, Bm = ps_mm[cb % 2], B_mm[cb % 2]
                        for kc in range(8):
                            k.op("pe", lambda kc=kc, cb=cb, mm=mm: P.matmul(mm[:], hT[:, kc, :], w_in_bf[:, kc, cb * 512:(cb + 1) * 512], start=(kc == 0), stop=(kc == 7)),
                                 reads=[B["hT"]] + CW, writes=[Bm], inc=(kc == 7))
                        if cb == 0:
                            k.op("dve", lambda mm=mm: V.tensor_copy(out=u_bf[s_][:], in_=mm[:]), reads=[Bm], writes=[B_u[s_]])
                        elif cb in (1, 2):
                            o8 = 0 if cb == 1 else 8
                            k.op("act", lambda mm=mm: S.activation(out=sq, in_=mm[:], func=AF.Square), reads=[Bm], writes=[B["t1"]])
                            k.op("dve", lambda o8=o8: V.tensor_reduce(out=qkst[:, o8:o8 + 8], in_=sq.rearrange("p (a b) -> p a b", b=64), axis=AX.X, op=ALU.add),
                                 reads=[B["t1"]], writes=[B["qkst"]])
                            k.op("act", lambda o8=o8: S.activation(out=qkst[:, o8:o8 + 8], in_=qkst[:, o8:o8 + 8], func=AF.Ln, scale=1.0 / 64, bias=eps_col[:, 0:1]), reads=[B["qkst"]], writes=[B["qkst"]])
                            k.op("act", lambda o8=o8: S.activation(out=qkst[:, o8:o8 + 8], in_=qkst[:, o8:o8 + 8], func=AF.Exp, scale=-0.5), reads=[B["qkst"]], writes=[B["qkst"]])
                            rb = qkst[:, o8:o8 + 8].unsqueeze(2).to_broadcast([128, 8, 64])
                            if cb == 1:
                                k.op("dve", lambda mm=mm, rb=rb: V.tensor_tensor(out=qtmp.rearrange("p (a b) -> p a b", b=64), in0=mm[:].rearrange("p (a b) -> p a b", b=64), in1=rb, op=ALU.mult),
                                     reads=[Bm, B["qkst"]], writes=[B["t2"]])
                                k.op("dve", lambda: V.tensor_tensor(out=qn_bf[:].rearrange("p (a b) -> p a b", b=64), in0=qtmp.rearrange("p (a b) -> p a b", b=64),
                                                                    in1=gq_bc[:].unsqueeze(1).to_broadcast([128, 8, 64]), op=ALU.mult),
                                     reads=[B["t2"]] + CW, writes=[B["qn"]])
                            else:
                                k.op("dve", lambda mm=mm, rb=rb: V.tensor_tensor(out=kn_bf[:].rearrange("p (a b) -> p a b", b=64), in0=mm[:].rearrange("p (a b) -> p a b", b=64), in1=rb, op=ALU.mult),
                                     reads=[Bm, B["qkst"]], writes=[B["kn"]])
                        elif cb == 3:
                            k.op("act", lambda mm=mm: S.copy(out=v_aug[:, t, :, 0:128], in_=mm[:].rearrange("p (a b) -> p a b", b=128)), reads=[Bm], writes=[B["vaug"]])
                        elif cb in (4, 5):
                            k.op("act", lambda mm=mm, cb=cb: S.activation(out=gp_sb[:, (cb - 4) * 512:(cb - 3) * 512], in_=mm[:], func=AF.Sigmoid), reads=[Bm], writes=[B["gp"]])
                        else:
                            k.op("act", lambda mm=mm, cb=cb: S.activation(out=ga_sb[:, (cb - 6) * 512:(cb - 5) * 512], in_=mm[:], func=AF.Sigmoid), reads=[Bm], writes=[B["ga"]])
                    if stop_at == 2:
                        raise _StopTile
                    for hh in range(4):
                        k.op("pe", lambda hh=hh: P.transpose(ps_tp[:, hh * 128:(hh + 1) * 128], qn_bf[:, hh * 128:(hh + 1) * 128], ident_bf[:]),
                             reads=[B["qn"]] + CW, writes=[B["pstp"]], inc=False)
                    for hh in range(4):
                        k.op("pe", lambda hh=hh: P.transpose(ps_tp[:, 512 + hh * 128:512 + (hh + 1) * 128], kn_bf[:, hh * 128:(hh + 1) * 128], ident_bf[:]),
                             reads=[B["kn"]] + CW, writes=[B["pstp"]], inc=(hh == 3))
                    k.op("dve", lambda: V.tensor_copy(out=qT[:].rearrange("p a b -> p (a b)"), in_=ps_tp[:, 0:512]), reads=[B["pstp"]], writes=[B["qT"]])
                    for hh in range(4):
                        k.op("dve", lambda hh=hh: V.tensor_copy(out=kT[:, hh, t * 128:(t + 1) * 128], in_=ps_tp[:, 512 + hh * 128:512 + (hh + 1) * 128]), reads=[B["pstp"]], writes=[B["kT"]])
                    if stop_at == 30:
                        raise _StopTile
                    for g in range(4):
                        aidx = g if t == 0 else 4 + g
                        k.op("pe", lambda g=g, aidx=aidx: P.matmul(ps_misc[:, g * 128:(g + 1) * 128], u_bf[s_][:, g * 128:(g + 1) * 128], poolA_bf[:, aidx, :], start=True, stop=(t == 0)),
                             reads=[B_u[s_]] + CW, writes=[B["psmisc"]], inc=(t == 0 and g == 3))
                        if t > 0:
                            k.op("pe", lambda g=g: P.matmul(ps_misc[:, g * 128:(g + 1) * 128], u_bf[1 - s_][:, g * 128:(g + 1) * 128], poolA_bf[:, 8 + g, :], start=False, stop=True),
                                 reads=[B_u[1 - s_]] + CW, writes=[B["psmisc"]], inc=(g == 3))
                    k.op("dve", lambda: V.tensor_copy(out=dT_bf[:], in_=ps_misc[:]), reads=[B["psmisc"]], writes=[B["dT"]])
                    if stop_at == 31:
                        raise _StopTile
                    for g in range(4):
                        k.op("pe", lambda g=g: P.matmul(ps_misc[:, g * 128:(g + 1) * 128], poolw_bf[:, g, :], dT_bf[:, g * 128:(g + 1) * 128], start=True, stop=True),
                             reads=[B["dT"]] + CW, writes=[B["psmisc"]], inc=(g == 3))
                    k.op("dve", lambda: V.tensor_tensor(out=yT_bf[:].rearrange("p (a b) -> p a b", b=128), in0=ps_misc[:].rearrange("p (a b) -> p a b", b=128),
                                                        in1=pscale[:].unsqueeze(2).to_broadcast([128, 4, 128]), op=ALU.mult),
                         reads=[B["psmisc"]] + CW, writes=[B["yT"]])
                    if stop_at == 32:
                        raise _StopTile
                    for nh in range(2):
                        for g in range(4):
                            k.op("pe", lambda g=g, nh=nh: P.matmul(ps_mm[nh][:], yT_bf[:, g * 128:(g + 1) * 128], wpu_bf[:, g, nh * 512:(nh + 1) * 512], start=(g == 0), stop=(g == 3)),
                                 reads=[B["yT"]] + CW, writes=[B_mm[nh]], inc=(g == 3))
                        k.op("dve", lambda nh=nh: V.tensor_tensor(out=t1[:, nh * 512:(nh + 1) * 512], in0=ps_mm[nh][:], in1=gp_sb[:, nh * 512:(nh + 1) * 512], op=ALU.mult),
                             reads=[B["gp"], B_mm[nh]], writes=[B["t1"]])
                    if stop_at == 3:
                        raise _StopTile
                    chunks = []
                    for hh in range(4):
                        for m in range(2):
                            for c0 in range(0, t + 1, 4):
                                chunks.append((hh, m, list(range(c0, min(c0 + 4, t + 1)))))

                    def emit_scores(ci):
                        hh, m, jl = chunks[ci]
                        bk = ci % 2
                        for q_, j in enumerate(jl):
                            k.op("pe", lambda: P.matmul(ps_sc[bk][:, q_ * 128:(q_ + 1) * 128], kT[64 * m:64 * m + 64, hh, j * 128:(j + 1) * 128], qT[64 * m:64 * m + 64, hh, :], start=True, stop=True),
                                 reads=[B["kT"], B["qT"]], writes=[B_sc[bk]], inc=(q_ == len(jl) - 1))

                    emit_scores(0)
                    ptc = 0
                    for ci, (hh, m, jl) in enumerate(chunks):
                        if ci + 1 < len(chunks):
                            emit_scores(ci + 1)
                        bk = ci % 2
                        po, Bpo = ps_o[hh % 2], B_pso[hh % 2]
                        for q_, j in enumerate(jl):
                            pr = ptc % NPT
                            ptc += 1
                            dlt = t - j
                            k.op("act", lambda: S.activation(out=PT[pr][:], in_=ps_sc[bk][:, q_ * 128:(q_ + 1) * 128], func=AF.Exp, bias=alibi[:, hh * 16 + dlt:hh * 16 + dlt + 1], scale=1.0),
                                 reads=[B_sc[bk]] + CW, writes=[B_PT[pr]])
                            if j == t:
                                k.op("pool", lambda: G.tensor_tensor(out=PT[pr][:], in0=PT[pr][:], in1=cmask_bf[:], op=ALU.mult), reads=[B_PT[pr]] + CW, writes=[B_PT[pr]])
                            k.op("pe", lambda: P.matmul(po[:, m * 129:(m + 1) * 129], PT[pr][:], v_aug[:, j, hh, 0:129], start=(j == 0), stop=(j == t)),
                                 reads=[B_PT[pr], B["vaug"]], writes=[Bpo], inc=(j == t))
                        if m == 1 and jl[-1] == t:
                            pov = po[:, 0:258].rearrange("p (a b) -> p a b", b=129)
                            k.op("dve", lambda: V.reciprocal(out=ost[:, 0:2].unsqueeze(2), in_=pov[:, :, 128:129]), reads=[Bpo], writes=[B["ost"]])
                            k.op("dve", lambda: V.tensor_tensor(out=ost[:, 2:3], in0=ost[:, 1:2], in1=nlam, op=ALU.mult), reads=[B["ost"]] + CW, writes=[B["ost"]])
                            k.op("dve", lambda: V.tensor_scalar(out=o_all[:, hh, :], in0=po[:, 0:128], scalar1=ost[:, 0:1], scalar2=None, op0=ALU.mult),
                                 reads=[Bpo, B["ost"]], writes=[B["oall"]])
                            k.op("dve", lambda: V.tensor_scalar(out=otmp[:], in0=po[:, 129:257], scalar1=ost[:, 2:3], scalar2=None, op0=ALU.mult), reads=[Bpo, B["ost"]], writes=[B["otmp"]])
                            k.op("dve", lambda: V.tensor_tensor(out=o_all[:, hh, :], in0=o_all[:, hh, :], in1=otmp[:], op=ALU.add), reads=[B["otmp"], B["oall"]], writes=[B["oall"]])
                    if stop_at == 4:
                        raise _StopTile
                    for hh in range(4):
                        k.op("act", lambda hh=hh: S.activation(out=junk[:, 0:128], in_=o_all[:, hh, :], func=AF.Square, accum_out=ost[:, 4 + hh:5 + hh]),
                             reads=[B["oall"]], writes=[B["ost"], B["mix"]])
                    k.op("act", lambda: S.activation(out=ost[:, 8:12], in_=ost[:, 4:8], func=AF.Ln, scale=1.0 / 128, bias=eps_col[:, 0:1]), reads=[B["ost"]], writes=[B["ost"]])
                    k.op("act", lambda: S.activation(out=ost[:, 8:12], in_=ost[:, 8:12], func=AF.Exp, scale=-0.5), reads=[B["ost"]], writes=[B["ost"]])
                    for hh in range(4):
                        k.op("dve", lambda hh=hh: V.tensor_scalar(out=otmp[:], in0=o_all[:, hh, :], scalar1=ost[:, 8 + hh:9 + hh], scalar2=None, op0=ALU.mult), reads=[B["oall"], B["ost"]], writes=[B["otmp"]])
                        k.op("dve", lambda hh=hh: V.tensor_tensor(out=o_n[:, hh * 128:(hh + 1) * 128], in0=otmp[:], in1=gsub_bc[:], op=ALU.mult), reads=[B["otmp"]] + CW, writes=[B["on"]])
                    for hh in range(4):
                        k.op("pe", lambda hh=hh: P.transpose(ps_tp[:, hh * 128:(hh + 1) * 128], o_n[:, hh * 128:(hh + 1) * 128], ident_bf[:]),
                             reads=[B["on"]] + CW, writes=[B["pstp"]], inc=(hh == 3))
                    k.op("dve", lambda: V.tensor_copy(out=o_nT[:].rearrange("p a b -> p (a b)"), in_=ps_tp[:, 0:512]), reads=[B["pstp"]], writes=[B["onT"]])
                    for nh in range(2):
                        for hh in range(4):
                            k.op("pe", lambda hh=hh, nh=nh: P.matmul(ps_mm[nh][:], o_nT[:, hh, :], wau_bf[:, hh, nh * 512:(nh + 1) * 512], start=(hh == 0), stop=(hh == 3)),
                                 reads=[B["onT"]] + CW, writes=[B_mm[nh]], inc=(hh == 3))
                        k.op("dve", lambda nh=nh: V.tensor_tensor(out=t2[:, nh * 512:(nh + 1) * 512], in0=ps_mm[nh][:], in1=ga_sb[:, nh * 512:(nh + 1) * 512], op=ALU.mult),
                             reads=[B["ga"], B_mm[nh]], writes=[B["t2"]])
                    k.op("pool", lambda: G.tensor_tensor(out=mix_bf[:], in0=t1[:], in1=t2[:], op=ALU.add), reads=[B["t1"], B["t2"]], writes=[B["mix"]])
                    for kc in range(8):
                        k.op("pe", lambda kc=kc: P.transpose(ps_tp[:, kc * 128:(kc + 1) * 128], mix_bf[:, kc * 128:(kc + 1) * 128], ident_bf[:]),
                             reads=[B["mix"]] + CW, writes=[B["pstp"]], inc=(kc == 7))
                    k.op("act", lambda: S.copy(out=mixT[:].rearrange("p a b -> p (a b)"), in_=ps_tp[:]), reads=[B["pstp"]], writes=[B["mixT"]])
                    x1s, Bx1 = x1[s_], B_x1[s_]
                    for nh in range(2):
                        for kc in range(8):
                            k.op("pe", lambda kc=kc, nh=nh: P.matmul(ps_mm[nh][:], mixT[:, kc, :], w_out_bf[:, kc, nh * 512:(nh + 1) * 512], start=(kc == 0), stop=(kc == 7)),
                                 reads=[B["mixT"]] + CW, writes=[B_mm[nh]], inc=(kc == 7))
                        k.op("dve", lambda nh=nh: V.tensor_tensor(out=x1s[:, nh * 512:(nh + 1) * 512], in0=ps_mm[nh][:], in1=xs[:, nh * 512:(nh + 1) * 512], op=ALU.add),
                             reads=[Bx, B_mm[nh]], writes=[Bx1])
                    k.dma("sp", f"xs{s_}", lambda: T.dma_start(out=out[Tg * 128:(Tg + 1) * 128, :], in_=x1s[:]), reads=[Bx1])
                    if stop_at == 5:
                        raise _StopTile
                    k.op("act", lambda: S.activation(out=junk[:], in_=x1s[:], func=AF.Square, accum_out=st1[:, 2:3]), reads=[Bx1], writes=[B["st1"], B["mix"]])
                    k.op("act", lambda: S.activation(out=st1[:, 3:4], in_=st1[:, 2:3], func=AF.Ln, scale=1.0 / D, bias=eps_col[:, 0:1]), reads=[B["st1"]], writes=[B["st1"]])
                    k.op("act", lambda: S.activation(out=st1[:, 3:4], in_=st1[:, 3:4], func=AF.Exp, scale=-0.5), reads=[B["st1"]], writes=[B["st1"]])
                    k.op("act", lambda: S.activation(out=t2[:], in_=x1s[:], func=AF.Copy, scale=st1[:, 3:4]), reads=[Bx1, B["st1"]], writes=[B["t2"]])
                    k.op("dve", lambda: V.tensor_tensor(out=hn_bf[s_][:], in0=t2[:], in1=g2_bc[:], op=ALU.mult), reads=[B["t2"]] + CW, writes=[B_hn[s_]])
                    ps_lg = ps_o[0][:, 300:336]
                    ps_rk = ps_o[1][:, 300:332]
                    for half in range(2):
                        for c4 in range(4):
                            kc = half * 4 + c4
                            k.op("pe", lambda kc=kc, c4=c4: P.transpose(ps_tpf[:, c4 * 128:(c4 + 1) * 128], x1s[:, kc * 128:(kc + 1) * 128], ident_f[:]),
                                 reads=[Bx1] + CW, writes=[B["psmisc"]], inc=(c4 == 3))
                        k.op("act", lambda: S.copy(out=x1T[:].rearrange("p a b -> p (a b)"), in_=ps_tpf[:]), reads=[B["psmisc"]], writes=[B["x1T"]])
                        for c4 in range(4):
                            kc = half * 4 + c4
                            k.op("pe", lambda kc=kc, c4=c4: P.matmul(ps_lg, x1T[:, c4, :], wr_f[:, kc, :], start=(kc == 0), stop=(kc == 7)),
                                 reads=[B["x1T"]] + CW, writes=[B["pslg"]], inc=(c4 == 3))
                    k.op("dve", lambda: V.tensor_scalar(out=L[:], in0=ps_lg, scalar1=st1[:, 3:4], scalar2=None, op0=ALU.mult), reads=[B["pslg"], B["st1"]], writes=[B["L"]])
                    k.op("dve", lambda: V.tensor_tensor(out=L[:], in0=L[:], in1=rbias[:], op=ALU.add), reads=[B["L"]] + CW, writes=[B["L"]])
                    if stop_at == 6:
                        raise _StopTile
                    R = [B["rt"]]
                    k.op("dve", lambda: V.tensor_reduce(out=rt[:, 0:1], in_=L[:, 0:4], axis=AX.X, op=ALU.max), reads=[B["L"]], writes=R)
                    k.op("dve", lambda: V.tensor_scalar(out=rt[:, 1:2], in0=rt[:, 0:1], scalar1=-1.0, scalar2=None, op0=ALU.mult), reads=R, writes=R)
                    k.op("act", lambda: S.activation(out=gexp[:], in_=L[:, 0:4], func=AF.Exp, bias=rt[:, 1:2], scale=1.0, accum_out=rt[:, 2:3]), reads=[B["L"]] + R, writes=[B["gexp"]] + R)
                    k.op("dve", lambda: V.reciprocal(out=rt[:, 3:4], in_=rt[:, 2:3]), reads=R, writes=R)
                    k.op("dve", lambda: V.tensor_scalar(out=gone[:], in0=L[:, 0:4], scalar1=rt[:, 0:1], scalar2=None, op0=ALU.is_equal), reads=[B["L"]] + R, writes=[B["gone"]])
                    k.op("dve", lambda: V.tensor_scalar(out=gone[:], in0=gone[:], scalar1=BIG, scalar2=-BIG, op0=ALU.mult, op1=ALU.add), reads=[B["gone"]], writes=[B["gone"]])
                    k.op("dve", lambda: V.tensor_tensor(out=Lm[:].rearrange("p (a b) -> p a b", b=8), in0=L[:, 4:36].rearrange("p (a b) -> p a b", b=8),
                                                        in1=gone[:].unsqueeze(2).to_broadcast([128, 4, 8]), op=ALU.add), reads=[B["L"], B["gone"]], writes=[B["Lm"]])
                    k.op("dve", lambda: V.tensor_reduce(out=rt[:, 4:5], in_=Lm[:], axis=AX.X, op=ALU.max), reads=[B["Lm"]], writes=R)
                    k.op("dve", lambda: V.tensor_scalar(out=one1[:], in0=Lm[:], scalar1=rt[:, 4:5], scalar2=None, op0=ALU.is_equal), reads=[B["Lm"]] + R, writes=[B["one1"]])
                    k.op("dve", lambda: V.tensor_scalar(out=Lm2[:], in0=one1[:], scalar1=-BIG, scalar2=None, op0=ALU.mult), reads=[B["one1"]], writes=[B["Lm2"]])
                    k.op("dve", lambda: V.tensor_tensor(out=Lm2[:], in0=Lm2[:], in1=Lm[:], op=ALU.add), reads=[B["Lm2"], B["Lm"]], writes=[B["Lm2"]])
                    k.op("dve", lambda: V.tensor_reduce(out=rt[:, 5:6], in_=Lm2[:], axis=AX.X, op=ALU.max), reads=[B["Lm2"]], writes=R)
                    k.op("dve", lambda: V.tensor_scalar(out=one2[:], in0=Lm2[:], scalar1=rt[:, 5:6], scalar2=None, op0=ALU.is_equal), reads=[B["Lm2"]] + R, writes=[B["one2"]])
                    k.op("dve", lambda: V.tensor_tensor(out=rt[:, 6:7], in0=rt[:, 4:5], in1=rt[:, 5:6], op=ALU.subtract), reads=R, writes=R)
                    k.op("act", lambda: S.activation(out=rt[:, 7:8], in_=rt[:, 6:7], func=AF.Sigmoid), reads=R, writes=R)
                    k.op("dve", lambda: V.tensor_tensor(out=rt[:, 8:9], in0=rt[:, 7:8], in1=rt[:, 3:4], op=ALU.mult), reads=R, writes=R)
                    k.op("dve", lambda: V.tensor_tensor(out=rt[:, 9:10], in0=rt[:, 3:4], in1=rt[:, 8:9], op=ALU.subtract), reads=R, writes=R)
                    k.op("dve", lambda: V.tensor_tensor(out=oh_bf[:], in0=one1[:], in1=one2[:], op=ALU.add), reads=[B["one1"], B["one2"]], writes=[B["oh"]])
                    k.op("pe", lambda: P.matmul(ps_rk, ustrict_bf[:], oh_bf[:], start=True, stop=False), reads=[B["oh"]] + CW, writes=[B["psrk"]], inc=False)
                    k.op("pe", lambda: P.matmul(ps_rk, ones_bf[:], cum_bf[:], start=False, stop=True), reads=[B["cum"]] + CW, writes=[B["psrk"]])
                    k.op("dve", lambda: V.tensor_copy(out=rk[:], in_=ps_rk), reads=[B["psrk"]], writes=[B["rk"]])
                    k.op("dve", lambda: V.tensor_tensor(out=cum_bf[:], in0=cum_bf[:], in1=oh_bf[:], op=ALU.add), reads=[B["cum"], B["oh"]], writes=[B["cum"]])
                    for kk_, oneX in ((0, one1), (1, one2)):
                        Bo = B["one1"] if kk_ == 0 else B["one2"]
                        c0 = 10 + 3 * kk_
                        k.op("dve", lambda oneX=oneX: V.tensor_tensor(out=tmp32[:], in0=oneX[:], in1=rk[:], op=ALU.mult), reads=[Bo, B["rk"]], writes=[B["tmp32"]])
                        k.op("dve", lambda c0=c0: V.tensor_reduce(out=rt[:, c0:c0 + 1], in_=tmp32[:], axis=AX.X, op=ALU.add), reads=[B["tmp32"]], writes=R)
                        k.op("dve", lambda oneX=oneX: V.tensor_tensor(out=tmp32[:], in0=oneX[:], in1=eoff[:], op=ALU.mult), reads=[Bo] + CW, writes=[B["tmp32"]])
                        k.op("dve", lambda c0=c0: V.tensor_reduce(out=rt[:, c0 + 1:c0 + 2], in_=tmp32[:], axis=AX.X, op=ALU.add), reads=[B["tmp32"]], writes=R)
                        k.op("dve", lambda c0=c0: V.tensor_scalar(out=rt[:, c0 + 2:c0 + 3], in0=rt[:, c0:c0 + 1], scalar1=float(CAP) - 0.5, scalar2=None, op0=ALU.is_lt), reads=R, writes=R)
                        k.op("dve", lambda c0=c0, kk_=kk_: V.tensor_tensor(out=w_all[:, 2 * Tg + kk_:2 * Tg + kk_ + 1], in0=rt[:, 8 + kk_:9 + kk_], in1=rt[:, c0 + 2:c0 + 3], op=ALU.mult),
                             reads=R, writes=[B_wall[Tg]])
                        k.op("dve", lambda c0=c0: V.tensor_scalar(out=rt[:, c0 + 2:c0 + 3], in0=rt[:, c0 + 2:c0 + 3], scalar1=-1.0e6, scalar2=1.0e6, op0=ALU.mult, op1=ALU.add), reads=R, writes=R)
                        k.op("dve", lambda c0=c0: V.tensor_tensor(out=rt[:, c0:c0 + 1], in0=rt[:, c0:c0 + 1], in1=rt[:, c0 + 1:c0 + 2], op=ALU.add), reads=R, writes=R)
                        k.op("dve", lambda c0=c0, kk_=kk_: V.tensor_tensor(out=slotf[:, kk_:kk_ + 1], in0=rt[:, c0:c0 + 1], in1=rt[:, c0 + 2:c0 + 3], op=ALU.add), reads=R, writes=[B["slotf"]])
                    k.op("dve", lambda: V.tensor_copy(out=slot_all[:, 2 * Tg:2 * Tg + 2], in_=slotf[:]), reads=[B["slotf"]], writes=[B_slot[Tg]])
                    for kk_ in range(2):
                        k.dma("pool", f"sc{s_}", lambda kk_=kk_: G.indirect_dma_start(out=xslots, out_offset=bass.IndirectOffsetOnAxis(ap=slot_all[:, 2 * Tg + kk_:2 * Tg + kk_ + 1], axis=0),
                                                                                     in_=hn_bf[s_][:, :], in_offset=None, bounds_check=bc_reg, oob_is_err=False),
                              reads=[B_hn[s_], B_slot[Tg]])
            except _StopTile:
                pass
            k.barrier()

        with ExitStack() as es:
          if "B" in phases:
              wg_bf = [sbt(es, f"wg_bf{i}", [128, 8, DE], BF16) for i in range(2)]
              wu_bf = [sbt(es, f"wu_bf{i}", [128, 8, DE], BF16) for i in range(2)]
              wd_bf = [sbt(es, f"wd_bf{i}", [128, 4, D], BF16) for i in range(2)]
              wg_f = [sbt(es, f"wg_f{i}", [128, 8, DE], F32) for i in range(2)]
              wu_f = [sbt(es, f"wu_f{i}", [128, 8, DE], F32) for i in range(2)]
              wd_f = [sbt(es, f"wd_f{i}", [128, 4, D], F32) for i in range(2)]
              Bst = [Buf("st0"), Buf("st1")]
              Bwp = [[Buf(f"wp{i}_{j}") for j in range(6)] for i in range(2)]
              xb = [sbt(es, f"xb{i}", [128, CT, D], BF16) for i in range(2)]
              xbT = sbt(es, "xbT", [128, 8, CAP], BF16)
              sg = [sbt(es, f"sg{i}", [128, 320], F32) for i in range(2)]
              hdnT = sbt(es, "hdnT", [128, 4, CAP], BF16)
              yt = [sbt(es, f"yt{i}", [128, D], F32) for i in range(2)]
              identb = sbt(es, "identb", [128, 128], BF16)
              pb_tp = [pst(es, f"pb_tp{i}", [128, D], BF16) for i in range(2)]
              pb_g = [pst(es, f"pb_g{i}", [128, 512], F32) for i in range(2)]
              pb_u = [pst(es, f"pb_u{i}", [128, 512], F32) for i in range(2)]
              pb_y = [pst(es, f"pb_y{i}", [128, 512], F32) for i in range(2)]
              Bw = [Buf("w0"), Buf("w1")]
              Bxb = [Buf("xb0"), Buf("xb1")]
              BxbT, BhdnT, Bid = Buf("xbT"), Buf("hdnT"), Buf("identb")
              Bsg = [Buf("sg0"), Buf("sg1")]
              Byt = [Buf("yt0"), Buf("yt1")]
              Bptp = [Buf("ptp0"), Buf("ptp1")]
              Bpg = [Buf("pg0"), Buf("pg1")]
              Bpu = [Buf("pu0"), Buf("pu1")]
              Bpy = [Buf("py0"), Buf("py1")]
              for s_ in ("es0", "es1", "xb0", "xb1", "ys0", "ys1", "idb"):
                  k.newsem(s_)
              k.dma("pool", "idb", lambda: G.dma_start(out=identb[:], in_=c_ident), writes=[Bid])

              def load_stage(e):
                  s_ = e % 2
                  k.dma("sp", f"es{s_}", lambda: T.dma_start(out=wg_f[s_][:], in_=w_eg[e].rearrange("(kc p) n -> p kc n", p=128)), writes=[Bst[s_]])
                  k.dma("sp", f"es{s_}", lambda: T.dma_start(out=wu_f[s_][:], in_=w_eu[e].rearrange("(kc p) n -> p kc n", p=128)))
                  k.dma("sp", f"es{s_}", lambda: T.dma_start(out=wd_f[s_][:], in_=w_ed[e].rearrange("(kc p) n -> p kc n", p=128)))
                  Bst[s_].w = (f"es{s_}", k.cnt[f"es{s_}"])

              def load_xb(e):
                  s_ = e % 2
                  k.dma("sp", f"xb{s_}", lambda: T.dma_start(out=xb[s_][:], in_=xslots[e * CAP:(e + 1) * CAP, :].rearrange("(st p) n -> p st n", p=128)), writes=[Bxb[s_]])

              def cast_piece(e, pc):
                  s_ = e % 2
                  src = (wg_f, wg_f, wu_f, wu_f, wd_f, wd_f)[pc][s_]
                  dst = (wg_bf, wg_bf, wu_bf, wu_bf, wd_bf, wd_bf)[pc][s_]
                  hsz = 4 if pc < 4 else 2
                  lo = (pc % 2) * hsz
                  if pc % 2 == 0:
                      k.op("dve", lambda: V.tensor_copy(out=dst[:, lo:lo + hsz, :], in_=src[:, lo:lo + hsz, :]), reads=[Bst[s_]], writes=[Bwp[s_][pc]])
                  else:
                      k.op("act", lambda: S.copy(out=dst[:, lo:lo + hsz, :], in_=src[:, lo:lo + hsz, :]), reads=[Bst[s_]], writes=[Bwp[s_][pc]])

              load_stage(0)
              load_xb(0)
              if NE > 1:
                  load_stage(1)
              for pc in range(6):
                  cast_piece(0, pc)
              ycount = 0
              for e in range(NE):
                  s_ = e % 2
                  if e + 1 < NE:
                      load_xb(e + 1)
                  pend = [(e + 1, pc) for pc in range(6)] if e + 1 < NE else []
                  for st in range(CT):
                      tp, Btp = pb_tp[st % 2], Bptp[st % 2]
                      for kc in range(8):
                          k.op("pe", lambda st=st, kc=kc, tp=tp: P.transpose(tp[:, kc * 128:(kc + 1) * 128], xb[s_][:, st, kc * 128:(kc + 1) * 128], identb[:]),
                               reads=[Bxb[s_], Bid], writes=[Btp], inc=(kc == 7))
                      eng = "act" if st % 2 == 0 else "dve"
                      if eng == "act":
                          k.op("act", lambda st=st, tp=tp: S.copy(out=xbT[:, :, st * 128:(st + 1) * 128], in_=tp[:].rearrange("p (a b) -> p a b", b=128)), reads=[Btp], writes=[BxbT])
                      else:
                          k.op("dve", lambda st=st, tp=tp: V.tensor_copy(out=xbT[:, :, st * 128:(st + 1) * 128], in_=tp[:].rearrange("p (a b) -> p a b", b=128)), reads=[Btp], writes=[BxbT])
                  if e + 2 < NE:
                      load_stage(e + 2)
                  for _ in range(2):
                      if pend:
                          cast_piece(*pend.pop(0))
                  i2 = 0
                  for fc in range(4):
                      for hf in range(2):
                          pg, pu = pb_g[i2 % 2], pb_u[i2 % 2]
                          Bg_, Bu_ = Bpg[i2 % 2], Bpu[i2 % 2]
                          sgi, Bsgi = sg[i2 % 2], Bsg[i2 % 2]
                          i2 += 1
                          for kc in range(8):
                              k.op("pe", lambda kc=kc, fc=fc, hf=hf, pg=pg: P.matmul(pg[:, 0:320], wg_bf[s_][:, kc, fc * 128:(fc + 1) * 128], xbT[:, kc, hf * 320:(hf + 1) * 320], start=(kc == 0), stop=(kc == 7)),
                                   reads=[Bwp[s_][kc // 4], BxbT], writes=[Bg_], inc=(kc == 7))
                          for kc in range(8):
                              k.op("pe", lambda kc=kc, fc=fc, hf=hf, pu=pu: P.matmul(pu[:, 0:320], wu_bf[s_][:, kc, fc * 128:(fc + 1) * 128], xbT[:, kc, hf * 320:(hf + 1) * 320], start=(kc == 0), stop=(kc == 7)),
                                   reads=[Bwp[s_][2 + kc // 4], BxbT], writes=[Bu_], inc=(kc == 7))
                          k.op("act", lambda pg=pg, sgi=sgi: S.activation(out=sgi[:], in_=pg[:, 0:320], func=AF.Silu), reads=[Bg_], writes=[Bsgi])
                          k.op("dve", lambda pu=pu, sgi=sgi, fc=fc, hf=hf: V.tensor_tensor(out=hdnT[:, fc, hf * 320:(hf + 1) * 320], in0=pu[:, 0:320], in1=sgi[:], op=ALU.mult),
                               reads=[Bsgi, Bu_], writes=[BhdnT])
                      if pend:
                          cast_piece(*pend.pop(0))
                  while pend:
                      cast_piece(*pend.pop(0))
                  for st in range(CT):
                      ys = ycount % 2
                      ycount += 1
                      for nh in range(2):
                          py, Bpy_ = pb_y[nh], Bpy[nh]
                          for fc in range(4):
                              k.op("pe", lambda fc=fc, st=st, nh=nh, py=py: P.matmul(py[:], hdnT[:, fc, st * 128:(st + 1) * 128], wd_bf[s_][:, fc, nh * 512:(nh + 1) * 512], start=(fc == 0), stop=(fc == 3)),
                                   reads=[BhdnT, Bwp[s_][4 + fc // 2]], writes=[Bpy_], inc=(fc == 3))
                          if nh == 0:
                              k.op("act", lambda py=py, ys=ys: S.copy(out=yt[ys][:, 0:512], in_=py[:]), reads=[Bpy_], writes=[Byt[ys]])
                          else:
                              k.op("dve", lambda py=py, ys=ys: V.tensor_copy(out=yt[ys][:, 512:1024], in_=py[:]), reads=[Bpy_], writes=[Byt[ys]])
                      r0 = e * CAP + st * 128
                      k.dma("sp", f"ys{ys}", lambda ys=ys, r0=r0: T.dma_start(out=ybuf[r0:r0 + 128, :], in_=yt[ys][:]), reads=[Byt[ys]])
              k.barrier()

        with ExitStack() as es:
          if "C" in phases:
              y1 = [sbt(es, f"y1_{i}", [128, D], F32) for i in range(2)]
              y2 = [sbt(es, f"y2_{i}", [128, D], F32) for i in range(2)]
              xr = [sbt(es, f"xr{i}", [128, D], F32) for i in range(2)]
              By1 = [Buf("y1_0"), Buf("y1_1")]
              By2 = [Buf("y2_0"), Buf("y2_1")]
              Bxr = [Buf("xr0"), Buf("xr1")]
              for s_ in ("ga0", "ga1", "gb0", "gb1", "xr0", "xr1", "os0", "os1"):
                  k.newsem(s_)
              for i in range(2):
                  k.op("dve", lambda i=i: V.memset(y1[i][:], 0.0), writes=[By1[i]])
                  k.op("dve", lambda i=i: V.memset(y2[i][:], 0.0), writes=[By2[i]])

              def load_c(Tg):
                  s_ = Tg % 2
                  k.dma("sp", f"xr{s_}", lambda: T.dma_start(out=xr[s_][:], in_=out[Tg * 128:(Tg + 1) * 128, :]), writes=[Bxr[s_]])
                  k.dma("pool", f"ga{s_}", lambda: G.indirect_dma_start(out=y1[s_][:, :], out_offset=None, in_=ybuf, in_offset=bass.IndirectOffsetOnAxis(ap=slot_all[:, 2 * Tg:2 * Tg + 1], axis=0),
                                                                        bounds_check=bc_reg, oob_is_err=False), reads=[B_slot[Tg]], writes=[By1[s_]])
                  k.dma("pool", f"gb{s_}", lambda: G.indirect_dma_start(out=y2[s_][:, :], out_offset=None, in_=ybuf, in_offset=bass.IndirectOffsetOnAxis(ap=slot_all[:, 2 * Tg + 1:2 * Tg + 2], axis=0),
                                                                        bounds_check=bc_reg, oob_is_err=False), reads=[B_slot[Tg]], writes=[By2[s_]])

              load_c(0)
              for Tg in range(NT):
                  s_ = Tg % 2
                  if Tg + 1 < NT:
                      load_c(Tg + 1)
                  k.op("dve", lambda: V.tensor_scalar(out=y1[s_][:], in0=y1[s_][:], scalar1=w_all[:, 2 * Tg:2 * Tg + 1], scalar2=None, op0=ALU.mult), reads=[By1[s_], B_wall[Tg]], writes=[By1[s_]])
                  k.op("dve", lambda: V.tensor_tensor(out=xr[s_][:], in0=xr[s_][:], in1=y1[s_][:], op=ALU.add), reads=[By1[s_], Bxr[s_]], writes=[Bxr[s_]])
                  k.op("dve", lambda: V.tensor_scalar(out=y2[s_][:], in0=y2[s_][:], scalar1=w_all[:, 2 * Tg + 1:2 * Tg + 2], scalar2=None, op0=ALU.mult), reads=[By2[s_], B_wall[Tg]], writes=[By2[s_]])
                  k.op("dve", lambda: V.tensor_tensor(out=xr[s_][:], in0=xr[s_][:], in1=y2[s_][:], op=ALU.add), reads=[By2[s_], Bxr[s_]], writes=[Bxr[s_]])
                  k.dma("sp", f"os{s_}", lambda: T.dma_start(out=out[Tg * 128:(Tg + 1) * 128, :], in_=xr[s_][:]), reads=[Bxr[s_]])
              k.barrier()
    k.close()
    return nc


def _consts():
    wins = (2, 4, 8, 16)
    A = np.zeros((12, 128, 128), np.float32)
    for g, w in enumerate(wins):
        for t in range(128):
            cnt = min(t + 1, w)
            for j in range(max(0, t - w + 1), t + 1):
                A[g, j, t] += 1.0 / cnt
            A[g, t, t] -= 1.0
            for j in range(t - w + 1, t + 1):
                if j >= 0:
                    A[4 + g, j, t] += 1.0 / w
                else:
                    A[8 + g, 128 + j, t] += 1.0 / w
            A[4 + g, t, t] -= 1.0
    slopes = [2.0 ** (-8.0 * (h + 1) / 4) for h in range(4)]
    al = np.zeros((128, 64), np.float32)
    kl = np.arange(128, dtype=np.float32)
    for h in range(4):
        for d in range(16):
            al[:, h * 16 + d] = slopes[h] * (kl - 127.0 - 128.0 * d)
    kk = np.arange(128)[:, None]
    qq = np.arange(128)[None, :]
    cmask = (kk <= qq).astype(np.float32)
    ustrict = (kk < qq).astype(np.float32)
    eoff = np.tile((np.arange(NE, dtype=np.float32) * CAP)[None, :], (128, 1))
    ident = np.eye(128, dtype=np.float32)
    return {"c_poolA": A, "c_alibi": al, "c_cmask": cmask, "c_ustrict": ustrict, "c_eoff": eoff, "c_ident": ident}


_NC = None


def kernel(**inputs):
    global _NC
    if _NC is None:
        _NC = build_program()
    f = lambda a: np.ascontiguousarray(np.asarray(a, dtype=np.float32))
    x = f(inputs["x"])
    shared = {
        "norm1_g": f(inputs["norm1_g"]).reshape(1, D),
        "w_in": f(inputs["w_in"]).reshape(D, IN_W),
        "pool_w": f(inputs["pool_w"]).reshape(4, 128, 128),
        "pool_scale": f(inputs["pool_scale"]).reshape(1, 512),
        "w_pool_up": f(inputs["w_pool_up"]).reshape(512, D),
        "q_norm_g": f(inputs["q_norm_g"]).reshape(1, 64),
        "k_norm_g": f(inputs["k_norm_g"]).reshape(1, 64),
        "lambda_q1": f(inputs["lambda_q1"]).reshape(1, 64),
        "lambda_k1": f(inputs["lambda_k1"]).reshape(1, 64),
        "lambda_q2": f(inputs["lambda_q2"]).reshape(1, 64),
        "lambda_k2": f(inputs["lambda_k2"]).reshape(1, 64),
        "subln_g": f(inputs["subln_g"]).reshape(1, 128),
        "w_attn_up": f(inputs["w_attn_up"]).reshape(512, D),
        "w_out": f(inputs["w_out"]).reshape(D, D),
        "norm2_g": f(inputs["norm2_g"]).reshape(1, D),
        "w_router_group": f(inputs["w_router_group"]).reshape(D, 4),
        "b_router_group": f(inputs["b_router_group"]).reshape(1, 4),
        "w_router_expert": f(inputs["w_router_expert"]).reshape(D, NE),
        "b_router_expert": f(inputs["b_router_expert"]).reshape(1, NE),
        "w_expert_gate": f(inputs["w_expert_gate"]).reshape(NE, D, DE),
        "w_expert_up": f(inputs["w_expert_up"]).reshape(NE, D, DE),
        "w_expert_down": f(inputs["w_expert_down"]).reshape(NE, DE, D),
    }
    shared.update(_consts())
    in_maps = []
    for c in range(N_CORES):
        m = dict(shared)
        m["x"] = np.ascontiguousarray(x[c * SEQ_PER_CORE:(c + 1) * SEQ_PER_CORE].reshape(NTOK, D))
        in_maps.append(m)
    res = run_bass_kernel_spmd(_NC, in_maps, core_ids=list(range(N_CORES)))
    outs = [np.asarray(r["out"], dtype=np.float32).reshape(SEQ_PER_CORE, SEQ, D) for r in res.results]
    return np.concatenate(outs, axis=0)
```
